# Optimizing a Trainium2 kernel written in Bass

```python
import math
import jax, jax.numpy as jnp
from jax import lax
import numpy as np

D_MODEL = 4096
BATCH = 4
SEQ = 2048
DEPTH = 1

D_ATTN = D_MODEL // 2
HEAD_DIM = 128
N_HEADS = D_ATTN // HEAD_DIM
MOBA_BLOCK = 256
MOBA_TOPK = 3
Q_CHUNK = 16
D_SSM = D_MODEL // 2
GROUP_CH = 16
N_GROUPS = D_SSM // GROUP_CH
STATE = 64
N_EXPERT_GROUPS = 8
EXPERTS_PER_GROUP = 8
N_EXPERTS = N_EXPERT_GROUPS * EXPERTS_PER_GROUP
TOPK_IN_GROUP = 2
D_EXPERT = D_MODEL // 8
EXPERT_ROWS = 128
D_PROJ = 3 * D_ATTN + D_SSM + 2 * D_MODEL
RMS_EPS = 1e-6
NEG = -1e30

kernel_name = 'hybrid_moba_s5_hmoe_block'


def rmsnorm(x, g):
    x32 = x.astype(jnp.float32)
    y = x32 * lax.rsqrt(jnp.mean(x32 * x32, axis=-1, keepdims=True) + RMS_EPS)
    return (y * g.astype(jnp.float32)).astype(x.dtype)


def alibi_slopes(n_heads):
    return jnp.exp2(-8.0 / n_heads * jnp.arange(1, n_heads + 1, dtype=jnp.float32))


def moba_alibi_attention(q, k, v):
    bsz, s = q.shape[0], q.shape[1]
    s_pad = -(-s // MOBA_BLOCK) * MOBA_BLOCK
    pad = s_pad - s
    q, k, v = [jnp.pad(t, ((0, 0), (0, pad), (0, 0), (0, 0))).transpose(0, 2, 1, 3) for t in (q, k, v)]
    nb = s_pad // MOBA_BLOCK
    n_sel = min(MOBA_TOPK, nb)
    k_blocks = k.reshape(bsz, N_HEADS, nb, MOBA_BLOCK, HEAD_DIM)
    v_blocks = v.reshape(bsz, N_HEADS, nb, MOBA_BLOCK, HEAD_DIM)
    k_mean = jnp.mean(k_blocks.astype(jnp.float32), axis=3)
    gate = jnp.einsum('bhsd,bhnd->bhsn', q.astype(jnp.float32), k_mean)
    q_blk = jnp.arange(s_pad) // MOBA_BLOCK
    fully_past = jnp.arange(nb)[None, :] < q_blk[:, None]
    gate = jnp.where(fully_past, gate, NEG)
    _, sel = lax.top_k(gate, n_sel)

    n_chunks = s_pad // Q_CHUNK
    q_c = q.reshape(bsz, N_HEADS, n_chunks, Q_CHUNK, HEAD_DIM).transpose(2, 0, 1, 3, 4)
    sel_c = sel.reshape(bsz, N_HEADS, n_chunks, Q_CHUNK, n_sel).transpose(2, 0, 1, 3, 4)
    slopes = alibi_slopes(N_HEADS)[None, :, None, None]
    scale = HEAD_DIM ** -0.5
    gather_blocks = jax.vmap(jax.vmap(lambda blocks, idx: blocks[idx]))

    def attend_chunk(args):
        qc, sc, c = args
        t = c * Q_CHUNK + jnp.arange(Q_CHUNK)
        i = (c * Q_CHUNK) // MOBA_BLOCK
        k_own = lax.dynamic_slice_in_dim(k, i * MOBA_BLOCK, MOBA_BLOCK, axis=2)
        v_own = lax.dynamic_slice_in_dim(v, i * MOBA_BLOCK, MOBA_BLOCK, axis=2)
        kp_own = i * MOBA_BLOCK + jnp.arange(MOBA_BLOCK)
        dist_own = (t[:, None] - kp_own[None, :]).astype(jnp.float32)
        l_own = jnp.einsum('bhqd,bhkd->bhqk', qc, k_own).astype(jnp.float32) * scale - slopes * dist_own
        l_own = jnp.where(dist_own >= 0, l_own, NEG)
        k_sel = gather_blocks(k_blocks, sc)
        v_sel = gather_blocks(v_blocks, sc)
        kp_sel = sc[..., None] * MOBA_BLOCK + jnp.arange(MOBA_BLOCK)
        dist_sel = (t[:, None, None] - kp_sel).astype(jnp.float32)
        l_sel = jnp.einsum('bhqd,bhqnkd->bhqnk', qc, k_sel).astype(jnp.float32) * scale - slopes[..., None] * dist_sel
        l_sel = jnp.where((sc < i)[..., None], l_sel, NEG)
        logits = jnp.concatenate([l_own, l_sel.reshape(bsz, N_HEADS, Q_CHUNK, n_sel * MOBA_BLOCK)], axis=-1)
        p = jax.nn.softmax(logits, axis=-1).astype(v.dtype)
        p_own = p[..., :MOBA_BLOCK]
        p_sel = p[..., MOBA_BLOCK:].reshape(bsz, N_HEADS, Q_CHUNK, n_sel, MOBA_BLOCK)
        return (jnp.einsum('bhqk,bhkd->bhqd', p_own, v_own)
                + jnp.einsum('bhqnk,bhqnkd->bhqd', p_sel, v_sel))

    out = lax.map(attend_chunk, (q_c, sel_c, jnp.arange(n_chunks)))
    out = out.transpose(1, 2, 0, 3, 4).reshape(bsz, N_HEADS, s_pad, HEAD_DIM)[:, :, :s]
    return out.transpose(0, 2, 1, 3).reshape(bsz, s, N_HEADS * HEAD_DIM)


def _complex_linear_combine(e1, e2):
    a1r, a1i, b1r, b1i = e1
    a2r, a2i, b2r, b2i = e2
    return (a2r * a1r - a2i * a1i,
            a2r * a1i + a2i * a1r,
            a2r * b1r - a2i * b1i + b2r,
            a2r * b1i + a2i * b1r + b2i)


def s5_branch(u, lam_re, lam_im, log_step, b_re, b_im, c_re, c_im, d_skip, w_glu):
    f32 = jnp.float32
    bsz, s, _ = u.shape
    ug = u.astype(f32).reshape(bsz, s, N_GROUPS, GROUP_CH)
    lr, li = lam_re.astype(f32), lam_im.astype(f32)
    dt = jnp.exp(log_step.astype(f32))[:, None]
    mag = jnp.exp(lr * dt)
    a_r, a_i = mag * jnp.cos(li * dt), mag * jnp.sin(li * dt)
    den = lr * lr + li * li
    f_r = ((a_r - 1.0) * lr + a_i * li) / den
    f_i = (a_i * lr - (a_r - 1.0) * li) / den
    br, bi = b_re.astype(f32), b_im.astype(f32)
    bb_r = f_r[..., None] * br - f_i[..., None] * bi
    bb_i = f_r[..., None] * bi + f_i[..., None] * br
    bu_r = jnp.einsum('bsgh,gph->sbgp', ug, bb_r)
    bu_i = jnp.einsum('bsgh,gph->sbgp', ug, bb_i)
    a_r_t = jnp.broadcast_to(a_r, (s, 1, N_GROUPS, STATE))
    a_i_t = jnp.broadcast_to(a_i, (s, 1, N_GROUPS, STATE))
    _, _, x_r, x_i = lax.associative_scan(_complex_linear_combine, (a_r_t, a_i_t, bu_r, bu_i), axis=0)
    y = (jnp.einsum('sbgp,ghp->bsgh', x_r, c_re.astype(f32))
         - jnp.einsum('sbgp,ghp->bsgh', x_i, c_im.astype(f32))
         + d_skip.astype(f32) * ug)
    y = jax.nn.gelu(y.reshape(bsz, s, D_SSM)).astype(u.dtype)
    za, zb = jnp.split(y @ w_glu, 2, axis=-1)
    return za * jax.nn.sigmoid(zb)


def hierarchical_moe(x, w_router_grp, b_router_grp, w_router_exp, b_router_exp, w_gate, w_up, w_down):
    bsz, s, d = x.shape
    n_tok = bsz * s
    xt = x.reshape(n_tok, d)
    lg = (xt @ w_router_grp).astype(jnp.float32) + b_router_grp.astype(jnp.float32)
    pg = jax.nn.softmax(lg, axis=-1)
    _, g_sel = lax.top_k(lg, 1)
    p_grp = jnp.take_along_axis(pg, g_sel, axis=-1)
    le = ((xt @ w_router_exp).astype(jnp.float32) + b_router_exp.astype(jnp.float32)).reshape(n_tok, N_EXPERT_GROUPS, EXPERTS_PER_GROUP)
    le_g = jnp.take_along_axis(le, g_sel[:, :, None], axis=1)[:, 0]
    top_v, top_j = lax.top_k(le_g, TOPK_IN_GROUP)
    weights = p_grp * jax.nn.softmax(top_v, axis=-1)
    eid = g_sel * EXPERTS_PER_GROUP + top_j

    n_asg = n_tok * TOPK_IN_GROUP
    eid_f = eid.reshape(n_asg).astype(jnp.int32)
    tok_f = jnp.repeat(jnp.arange(n_tok, dtype=jnp.int32), TOPK_IN_GROUP)
    w_f = weights.reshape(n_asg)
    e_s, tok_s, w_s = lax.sort((eid_f, tok_f, w_f), num_keys=1)
    counts = jnp.zeros((N_EXPERTS,), jnp.int32).at[eid_f].add(1)
    starts = jnp.cumsum(counts) - counts
    padded = (counts + EXPERT_ROWS - 1) // EXPERT_ROWS * EXPERT_ROWS
    pends = jnp.cumsum(padded)
    pstarts = pends - padded
    dest = pstarts[e_s] + (jnp.arange(n_asg, dtype=jnp.int32) - starts[e_s])
    n_blk = (n_asg + EXPERT_ROWS - 1) // EXPERT_ROWS + N_EXPERTS
    rows = n_blk * EXPERT_ROWS
    xs = jnp.zeros((rows, d), x.dtype).at[dest].set(xt[tok_s])
    blk_e = jnp.minimum(jnp.searchsorted(pends, jnp.arange(n_blk, dtype=jnp.int32) * EXPERT_ROWS, side='right'), N_EXPERTS - 1)

    def expert_rows(args):
        xb, e = args
        hdn = jax.nn.silu(xb @ w_gate[e]) * (xb @ w_up[e])
        return hdn @ w_down[e]

    ys = lax.map(expert_rows, (xs.reshape(n_blk, EXPERT_ROWS, d), blk_e)).reshape(rows, d)
    y_asg = ys[dest] * w_s[:, None].astype(ys.dtype)
    out = jnp.zeros((n_tok, d), ys.dtype).at[tok_s].add(y_asg)
    return out.reshape(bsz, s, d)


def setup_inputs(seed: int = 0) -> dict:
    key = jax.random.key(seed)
    ks = jax.random.split(key, 24)
    f32 = jnp.float32

    def nrm(k, shape, scale):
        return jax.random.normal(k, shape, f32) * scale

    return {
        'x': nrm(ks[0], (BATCH, SEQ, D_MODEL), 1.0),
        'g_mix': 1.0 + nrm(ks[1], (DEPTH, D_MODEL), 0.02),
        'w_in': nrm(ks[2], (DEPTH, D_MODEL, D_PROJ), D_MODEL ** -0.5),
        'ssm_lam_re': -0.5 + nrm(ks[3], (DEPTH, N_GROUPS, STATE), 0.01),
        'ssm_lam_im': math.pi * jnp.arange(STATE, dtype=f32) + nrm(ks[4], (DEPTH, N_GROUPS, STATE), 0.01),
        'ssm_log_step': jax.random.uniform(ks[5], (DEPTH, N_GROUPS), f32, math.log(1e-3), math.log(1e-1)),
        'ssm_b_re': nrm(ks[6], (DEPTH, N_GROUPS, STATE, GROUP_CH), (2 * GROUP_CH) ** -0.5),
        'ssm_b_im': nrm(ks[7], (DEPTH, N_GROUPS, STATE, GROUP_CH), (2 * GROUP_CH) ** -0.5),
        'ssm_c_re': nrm(ks[8], (DEPTH, N_GROUPS, GROUP_CH, STATE), STATE ** -0.5),
        'ssm_c_im': nrm(ks[9], (DEPTH, N_GROUPS, GROUP_CH, STATE), STATE ** -0.5),
        'ssm_d': nrm(ks[10], (DEPTH, N_GROUPS, GROUP_CH), 1.0),
        'w_glu': nrm(ks[11], (DEPTH, D_SSM, 2 * D_SSM), D_SSM ** -0.5),
        'w_o_attn': nrm(ks[12], (DEPTH, D_ATTN, D_MODEL), D_ATTN ** -0.5),
        'w_o_ssm': nrm(ks[13], (DEPTH, D_SSM, D_MODEL), D_SSM ** -0.5),
        'w_out': nrm(ks[14], (DEPTH, D_MODEL, D_MODEL), D_MODEL ** -0.5),
        'g_ffn': 1.0 + nrm(ks[15], (DEPTH, D_MODEL), 0.02),
        'w_router_grp': nrm(ks[16], (DEPTH, D_MODEL, N_EXPERT_GROUPS), D_MODEL ** -0.5),
        'b_router_grp': nrm(ks[17], (DEPTH, N_EXPERT_GROUPS), 0.01),
        'w_router_exp': nrm(ks[18], (DEPTH, D_MODEL, N_EXPERTS), D_MODEL ** -0.5),
        'b_router_exp': nrm(ks[19], (DEPTH, N_EXPERTS), 0.01),
        'w_gate': nrm(ks[20], (DEPTH, N_EXPERTS, D_MODEL, D_EXPERT), D_MODEL ** -0.5),
        'w_up': nrm(ks[21], (DEPTH, N_EXPERTS, D_MODEL, D_EXPERT), D_MODEL ** -0.5),
        'w_down': nrm(ks[22], (DEPTH, N_EXPERTS, D_EXPERT, D_MODEL), D_EXPERT ** -0.5),
        'g_final': 1.0 + nrm(ks[23], (D_MODEL,), 0.02),
    }


def reference(x, g_mix, w_in, ssm_lam_re, ssm_lam_im, ssm_log_step, ssm_b_re, ssm_b_im,
              ssm_c_re, ssm_c_im, ssm_d, w_glu, w_o_attn, w_o_ssm, w_out, g_ffn,
              w_router_grp, b_router_grp, w_router_exp, b_router_exp, w_gate, w_up, w_down, g_final):
    h = x
    bsz, s, _ = x.shape
    cuts = [D_ATTN, 2 * D_ATTN, 3 * D_ATTN, 3 * D_ATTN + D_SSM, 3 * D_ATTN + D_SSM + D_MODEL]
    for l in range(DEPTH):
        a = rmsnorm(h, g_mix[l])
        q, k, v, u, gate_attn, gate_ssm = jnp.split(a @ w_in[l], cuts, axis=-1)
        heads = lambda t: t.reshape(bsz, s, N_HEADS, HEAD_DIM)
        y_attn = moba_alibi_attention(heads(q), heads(k), heads(v))
        y_ssm = s5_branch(u, ssm_lam_re[l], ssm_lam_im[l], ssm_log_step[l], ssm_b_re[l], ssm_b_im[l],
                          ssm_c_re[l], ssm_c_im[l], ssm_d[l], w_glu[l])
        mixed = (jax.nn.sigmoid(gate_attn) * (y_attn @ w_o_attn[l])
                 + jax.nn.sigmoid(gate_ssm) * (y_ssm @ w_o_ssm[l]))
        h = h + mixed @ w_out[l]
        h = h + hierarchical_moe(rmsnorm(h, g_ffn[l]), w_router_grp[l], b_router_grp[l],
                                 w_router_exp[l], b_router_exp[l], w_gate[l], w_up[l], w_down[l])
    return rmsnorm(h, g_final)
```

```python
import contextlib
import math
import numpy as np
import ml_dtypes
import concourse.bass as bass
import concourse.mybir as mybir
from concourse.bass_utils import run_bass_kernel_spmd

F32 = mybir.dt.float32
BF16 = mybir.dt.bfloat16
AF = mybir.ActivationFunctionType
ALU = mybir.AluOpType
AX = mybir.AxisListType

D = 4096
TOK = 1024
TALL = 2048
NH = 16
DH = 128
G = 128
NE = 64
CAP = 64
NSLOT = NE * CAP
DE = 512
NDS = 20
NDS_SW = 8
SEM_LIMIT = 50000
SAME_ENGINE_SYNC = True
STOP_AFTER = 99
PHASES = {1, 2, 3, 4, 5, 6, 7}
INPUT_NAMES = ()


def _phase(n):
    if n in PHASES:
        with contextlib.ExitStack() as st:
            yield st


class Buf:
    __slots__ = ("w", "r")

    def __init__(self):
        self.w = None
        self.r = {}


class Tr:
    def __init__(self, nc, es):
        self.nc = nc
        self.es = es
        self.E = {"pe": nc.tensor, "act": nc.scalar, "dve": nc.vector, "pool": nc.gpsimd, "sp": nc.sync}
        self.sem = {}
        self.cnt = {}
        self.nsem = 0
        self.seen = {e: {} for e in self.E}
        for e in self.E:
            self._newsem(e)
        self.dsems = [es.enter_context(nc.semaphore(f"dma{i}")) for i in range(NDS + NDS_SW)]
        self.dcnt = [0] * (NDS + NDS_SW)
        self.dnext = 0
        self.dnext_sw = 0
        self.bufs = {}
        self.all_events = {}

    def _newsem(self, e):
        self.sem[e] = self.es.enter_context(self.nc.semaphore(f"s_{e}_{self.nsem}"))
        self.nsem += 1
        self.cnt[e] = 0

    def B(self, *key):
        b = self.bufs.get(key)
        if b is None:
            b = Buf()
            self.bufs[key] = b
        return b

    def _deps(self, R, W):
        evs = []
        for b in R:
            if b.w is not None:
                evs.append((b.w, True))
        for b in W:
            if b.w is not None:
                evs.append((b.w, False))
            evs.extend((x, False) for x in b.r.values())
        return evs

    def _wait(self, e, evs):
        need = {}
        for ((sem, val, eng), is_raw) in evs:
            if eng == e and (e == "pe" or e == "sp" or not SAME_ENGINE_SYNC or not is_raw):
                continue
            k = id(sem)
            if k not in need or need[k][1] < val:
                need[k] = (sem, val, eng)
        for k, (sem, val, eng) in need.items():
            if self.seen[e].get(k, 0) >= val:
                continue
            self.E[e].wait_ge(sem, val)
            self.seen[e][k] = val

    def _record(self, ev, R, W):
        k = id(ev[0])
        for b in R:
            b.r[k] = ev
        for b in W:
            b.w = ev
            b.r = {}
        self.all_events[k] = ev

    def op(self, e, fn, R=(), W=()):
        self._wait(e, self._deps(R, W))
        ins = fn()
        self.cnt[e] += 1
        ins.then_inc(self.sem[e], 1)
        ev = (self.sem[e], self.cnt[e], e)
        self._record(ev, R, W)
        if self.cnt[e] >= SEM_LIMIT:
            self._newsem(e)
        return ev

    def dma(self, q, out, in_, R=(), W=(), **kw):
        if q == "pool":
            i = NDS + self.dnext_sw
            self.dnext_sw = (self.dnext_sw + 1) % NDS_SW
        else:
            i = self.dnext
            self.dnext = (i + 1) % NDS
        sem = self.dsems[i]
        evs = self._deps(R, W)
        if self.dcnt[i] > 0:
            evs.append(((sem, self.dcnt[i], "dma"), True))
        self._wait(q, evs)
        self.E[q].dma_start(out=out, in_=in_, **kw).then_inc(sem, 16)
        self.dcnt[i] += 16
        ev = (sem, self.dcnt[i], "dma")
        self._record(ev, R, W)
        return ev

    def barrier(self):
        evs = [(x, True) for x in self.all_events.values()]
        for e in self.E:
            self._wait(e, evs)
        for b in self.bufs.values():
            b.w = None
            b.r = {}

    def final_wait(self):
        evs = [(x, True) for x in self.all_events.values()]
        self._wait("sp", evs)


def build(debug_names=()):
    nc = bass.Bass("TRN2", target_bir_lowering=False)

    def din(name, shape, dt=F32):
        return nc.dram_tensor(name, list(shape), dt, kind="ExternalInput").ap()

    def dscr(name, shape, dt):
        kind = "ExternalOutput" if name in debug_names else ("ExternalInput" if name in INPUT_NAMES else "Internal")
        return nc.dram_tensor(name, list(shape), dt, kind=kind).ap()

    xin = din("xin", [TALL, D])
    gmixT = din("gmixT", [128, 32])
    w_in = din("w_in", [D, 16384])
    w_glu = din("w_glu", [2048, 4096])
    w_oa = din("w_o_attn", [2048, 4096])
    w_os = din("w_o_ssm", [2048, 4096])
    w_out = din("w_out", [D, D])
    gffn = din("gffn", [128, D])
    gfin = din("gfin", [128, D])
    w_rt = din("w_rt", [D, 72])
    b_rt = din("b_rt", [128, 72])
    w_gate = din("w_gate", [NE, D, DE])
    w_up = din("w_up", [NE, D, DE])
    w_down = din("w_down", [NE, DE, D])
    s_lr = din("s_lr", [128, 16, 64])
    s_li = din("s_li", [128, 16, 64])
    s_ls = din("s_ls", [128, 16])
    s_br = din("s_br", [128, 16, 64])
    s_bi = din("s_bi", [128, 16, 64])
    s_d = din("s_d", [128, 16])
    s_crT = din("s_crT", [64, G * 16])
    s_ciT = din("s_ciT", [64, G * 16])
    s_liT = din("s_liT", [128, G])
    s_lrT = din("s_lrT", [128, G])
    s_lsT = din("s_lsT", [128, G])
    c_ident = din("c_ident", [128, 128], BF16)
    c_tb = din("c_tb", [128, 2048])
    c_lt = din("c_lt", [128, 128], BF16)
    c_ones = din("c_ones", [128, 128], BF16)
    c_iota = din("c_iota", [128, NSLOT])
    c_ecap = din("c_ecap", [128, NE])
    c_rowmask = din("c_rowmask", [128, 8])
    c_e16 = din("c_e16", [128, 16])
    c_sgn = din("c_sgn", [128, 1])
    c_t0 = din("c_t0", [128, 64])
    c_t1 = din("c_t1", [128, 32])
    c_tt = din("c_tt", [128, TALL])
    c_penG = din("c_penG", [128, 64])
    c_own = din("c_own", [128, 64])
    c_dis = din("c_dis", [128, 64])

    out = nc.dram_tensor("out", [TOK, D], F32, kind="ExternalOutput").ap()

    aT = dscr("aT", [D, TALL], BF16)
    qT = dscr("qT", [2048, TOK], BF16)
    kT = dscr("kT", [2048, TALL], BF16)
    vtm = dscr("vtm", [TALL, 2048], BF16)
    uT = dscr("uT", [2048, TALL], BF16)
    sga = dscr("sga", [D, TOK], BF16)
    sgs = dscr("sgs", [D, TOK], BF16)
    yattnT = dscr("yattnT", [2048, TOK], BF16)
    ypreT = dscr("ypreT", [2048, TOK], F32)
    ygT = dscr("ygT", [2048, TOK], BF16)
    yssmT = dscr("yssmT", [2048, TOK], BF16)
    mixedT = dscr("mixedT", [D, TOK], BF16)
    h1 = dscr("h1", [TOK, D], F32)
    hn = dscr("hn", [TOK, D], BF16)
    xs = dscr("xs", [NSLOT, D], BF16)
    ys = dscr("ys", [NSLOT, D], BF16)
    h2 = dscr("h2", [TOK, D], F32)
    rdbg = dscr("rdbg", [TOK, 8], F32)

    es = contextlib.ExitStack()
    with es:
        T = Tr(nc, es)
        PS = es.enter_context(nc.psum_tensor("PS", [128, 4096], F32))

        def bank(i):
            return PS[:, i * 512:(i + 1) * 512]

        def pb(i):
            return T.B("ps", i)

        def sb(name, shape, dt, stack):
            return stack.enter_context(nc.sbuf_tensor(name, list(shape), dt))

        ident = sb("ident", [128, 128], BF16, es)
        T.dma("sp", ident[:], c_ident, W=[T.B("ident")])
        ev_state = {"n": 0}

        def evac_engine():
            ev_state["n"] += 1
            return "act" if ev_state["n"] % 2 else "dve"

        def copy_op(eng, out_ap, in_ap, R, W, scale=None):
            if eng == "act":
                if scale is None:
                    T.op("act", lambda: nc.scalar.activation(out=out_ap, in_=in_ap, func=AF.Copy), R=R, W=W)
                else:
                    T.op("act", lambda: nc.scalar.activation(out=out_ap, in_=in_ap, func=AF.Copy, scale=scale), R=R, W=W)
            else:
                if scale is None:
                    T.op("dve", lambda: nc.vector.tensor_copy(out=out_ap, in_=in_ap), R=R, W=W)
                else:
                    T.op("dve", lambda: nc.vector.tensor_scalar(out=out_ap, in0=in_ap, scalar1=scale, scalar2=None, op0=ALU.mult), R=R, W=W)

        def rms_rstd(st, xt_ap, xbuf, junk, jbuf, ss, rs, tag):
            T.op("act", lambda: nc.scalar.activation(out=junk, in_=xt_ap, func=AF.Square, accum_out=ss),
                 R=[xbuf], W=[jbuf, T.B(tag, "ss")])
            T.op("act", lambda: nc.scalar.activation(out=rs, in_=ss, func=AF.Sqrt, scale=1.0 / D, bias=eps_t[:, 0:1]),
                 R=[T.B(tag, "ss"), T.B("eps")], W=[T.B(tag, "rs")])
            T.op("dve", lambda: nc.vector.reciprocal(out=rs, in_=rs), R=[T.B(tag, "rs")], W=[T.B(tag, "rs")])

        eps_t = sb("eps_t", [128, 1], F32, es)
        T.op("dve", lambda: nc.vector.memset(eps_t[:], 1e-6), W=[T.B("eps")])

        for ph in _phase(1):
            gm = sb("gm", [128, 32], F32, ph)
            T.dma("sp", gm[:], gmixT, W=[T.B("gm")])
            xt = [sb(f"p1x{i}", [128, D], F32, ph) for i in range(2)]
            xn = [sb(f"p1xn{i}", [128, D], BF16, ph) for i in range(2)]
            junk = sb("p1junk", [128, D], BF16, ph)
            ss = sb("p1ss", [128, 1], F32, ph)
            rs = sb("p1rs", [128, 1], F32, ph)
            agrp = [sb(f"p1ag{i}", [128, 32, 512], BF16, ph) for i in range(2)]
            for i in range(16):
                xb = T.B("p1x", i % 2)
                xnb = T.B("p1xn", i % 2)
                agb = T.B("p1ag", (i // 4) % 2)
                ag = agrp[(i // 4) % 2]
                T.dma("sp", xt[i % 2][:], xin[i * 128:(i + 1) * 128, :], W=[xb])
                rms_rstd(ph, xt[i % 2][:], xb, junk[:], T.B("p1junk"), ss[:], rs[:], "p1")
                T.op("dve", lambda: nc.vector.tensor_scalar(out=xn[i % 2][:], in0=xt[i % 2][:], scalar1=rs[:, 0:1], scalar2=None, op0=ALU.mult),
                     R=[xb, T.B("p1", "rs")], W=[xnb])
                for q4 in range(4):
                    bk = (i * 4 + q4) % 8
                    pview = bank(bk).bitcast(BF16)
                    for c8 in range(8):
                        c = q4 * 8 + c8
                        T.op("pe", lambda: nc.tensor.transpose(pview[:, c8 * 128:(c8 + 1) * 128], xn[i % 2][:, c * 128:(c + 1) * 128], ident[:]),
                             R=[xnb, T.B("ident")], W=[pb(bk)])
                    o_ap = ag[:, q4 * 8:(q4 + 1) * 8, (i % 4) * 128:(i % 4 + 1) * 128]
                    i_ap = pview.rearrange("p (c t) -> p c t", c=8)
                    g_ap = gm[:, q4 * 8:(q4 + 1) * 8].unsqueeze(2).broadcast_to([128, 8, 128])
                    T.op("dve", lambda: nc.vector.tensor_tensor(out=o_ap, in0=i_ap, in1=g_ap, op=ALU.mult),
                         R=[pb(bk), T.B("gm")], W=[agb])
                if i % 4 == 3:
                    gi = i // 4
                    T.dma("sp", aT.rearrange("(c p) t -> p c t", p=128)[:, :, gi * 512:(gi + 1) * 512], ag[:], R=[agb], W=[T.B("aT", gi)])
        T.barrier()
        if STOP_AFTER <= 1:
            T.final_wait()
            return nc

        def load_act(dst, dram_fm, K, t0, tn, bufkey):
            kc = K // 128
            half = kc // 2 if kc >= 2 else kc
            v = dram_fm.rearrange("(c p) t -> p c t", p=128)
            T.dma("sp", dst[:, 0:half, :], v[:, 0:half, t0:t0 + tn], W=[T.B(bufkey, 0)])
            if half < kc:
                T.dma("sp", dst[:, half:kc, :], v[:, half:kc, t0:t0 + tn], W=[T.B(bufkey, 1)])
            return [T.B(bufkey, 0), T.B(bufkey, 1)] if half < kc else [T.B(bufkey, 0)]

        def load_w(dst, wdram, K, c0, cn, bufkey):
            kc = K // 128
            v = wdram.rearrange("(c p) n -> p c n", p=128)
            step = max(1, kc // 4)
            bl = []
            for j, k0 in enumerate(range(0, kc, step)):
                T.dma("pool", dst[:, k0:k0 + step, :], v[:, k0:k0 + step, c0:c0 + cn], W=[T.B(bufkey, j)], max_dma_last_dim=4096)
                bl.append(T.B(bufkey, j))
            return bl

        SCALE = DH ** -0.5
        for ph in _phase(2):
            act = sb("p2act", [128, 32, TOK], BF16, ph)
            wbuf = [sb(f"p2w{i}", [128, 32, 512], BF16, ph) for i in range(2)]
            stg = [sb(f"p2s{i}", [128, TOK], BF16, ph) for i in range(2)]
            stv = [sb(f"p2v{i}", [128, 512], BF16, ph) for i in range(2)]
            nst = [0]
            for ps_ in range(2):
                tok0 = TOK if ps_ == 0 else 0
                actb = load_act(act, aT, D, tok0, TOK, "p2act")
                groups = list(range(32)) if ps_ == 0 else list(range(4, 16))
                for gi, cgp in enumerate(groups):
                    wb = wbuf[gi % 2]
                    wbl = load_w(wb, w_in, D, cgp * 512, 512, ("p2w", gi % 2))
                    kind = ["q", "k", "v", "u", "ga", "ga", "gs", "gs"][cgp // 4]
                    if kind == "v":
                        for tch in range(8):
                            bk = nst[0] % 8
                            for kc in range(32):
                                T.op("pe", lambda: nc.tensor.matmul(bank(bk), act[:, kc, tch * 128:(tch + 1) * 128], wb[:, kc, :], start=(kc == 0), stop=(kc == 31)),
                                     R=actb + wbl, W=[pb(bk)])
                            sv = stv[nst[0] % 2]
                            svb = T.B("p2v", nst[0] % 2)
                            copy_op(evac_engine(), sv[:], bank(bk), [pb(bk)], [svb])
                            col = (cgp - 8) * 512
                            T.dma("sp", vtm[tok0 + tch * 128: tok0 + (tch + 1) * 128, col:col + 512], sv[:], R=[svb], W=[T.B("vtm", tok0, tch, cgp)])
                            nst[0] += 1
                    else:
                        for nch in range(4):
                            st = stg[nst[0] % 2]
                            stb = T.B("p2s", nst[0] % 2)
                            for tt in range(2):
                                bk = (nst[0] * 2 + tt) % 8
                                for kc in range(32):
                                    T.op("pe", lambda: nc.tensor.matmul(bank(bk), wb[:, kc, nch * 128:(nch + 1) * 128], act[:, kc, tt * 512:(tt + 1) * 512], start=(kc == 0), stop=(kc == 31)),
                                         R=actb + wbl, W=[pb(bk)])
                                o_ap = st[:, tt * 512:(tt + 1) * 512]
                                if kind in ("ga", "gs"):
                                    T.op("act", lambda: nc.scalar.activation(out=o_ap, in_=bank(bk), func=AF.Sigmoid), R=[pb(bk)], W=[stb])
                                elif kind == "q":
                                    copy_op(evac_engine(), o_ap, bank(bk), [pb(bk)], [stb], scale=SCALE)
                                else:
                                    copy_op(evac_engine(), o_ap, bank(bk), [pb(bk)], [stb])
                            r0 = (cgp % 4) * 512 + nch * 128
                            if kind == "q":
                                dst = qT[r0:r0 + 128, :]
                            elif kind == "k":
                                dst = kT[r0:r0 + 128, tok0:tok0 + TOK]
                            elif kind == "u":
                                dst = uT[r0:r0 + 128, tok0:tok0 + TOK]
                            elif kind == "ga":
                                r0 = (cgp - 16) * 512 + nch * 128
                                dst = sga[r0:r0 + 128, :]
                            else:
                                r0 = (cgp - 24) * 512 + nch * 128
                                dst = sgs[r0:r0 + 128, :]
                            T.dma("sp", dst, st[:], R=[stb], W=[T.B("p2out", kind, cgp, nch, ps_)])
                            nst[0] += 1
        T.barrier()
        if STOP_AFTER <= 2:
            T.final_wait()
            return nc

        for ph in _phase(3):
            tb = sb("p3tb", [128, 2048], F32, ph)
            penG = sb("p3penG", [128, 64], F32, ph)
            own01 = sb("p3own", [128, 64], F32, ph)
            dis = sb("p3dis", [128, 64], F32, ph)
            T.dma("sp", tb[:], c_tb, W=[T.B("tb")])
            T.dma("sp", penG[:], c_penG, W=[T.B("penG")])
            T.dma("sp", own01[:], c_own, W=[T.B("own01")])
            T.dma("sp", dis[:], c_dis, W=[T.B("dis")])
            qh = [sb(f"p3q{i}", [128, TOK], BF16, ph) for i in range(2)]
            kh = [sb(f"p3k{i}", [128, TALL], BF16, ph) for i in range(2)]
            vh = [sb(f"p3v{i}", [128, 16, 128], BF16, ph) for i in range(2)]
            ksum = sb("p3ksum", [128, 8], F32, ph)
            ksb = sb("p3ksb", [128, 8], BF16, ph)
            gate = sb("p3gate", [128, 64], F32, ph)
            top8 = sb("p3top8", [128, 64], F32, ph)
            sel = sb("p3sel", [128, 64], F32, ph)
            biasb = sb("p3bias", [128, 64], F32, ph)
            ssb = [sb(f"p3ssb{i}", [128, 2048], F32, ph) for i in range(2)]
            pt = [sb(f"p3p{i}", [128, 2048], BF16, ph) for i in range(2)]
            ptT = sb("p3pT", [128, 16, 128], BF16, ph)
            rmax = [sb(f"p3rmax{i}", [128, 1], F32, ph) for i in range(2)]
            bq = [sb(f"p3bq{i}", [128, 8], F32, ph) for i in range(2)]
            sums = [sb(f"p3sums{i}", [128, 8], F32, ph) for i in range(2)]
            rsum = [sb(f"p3rsum{i}", [128, 1], F32, ph) for i in range(2)]
            on = sb("p3on", [128, 128], BF16, ph)
            yh = [sb(f"p3y{i}", [128, TOK], BF16, ph) for i in range(2)]

            def head_setup(h):
                hb = h % 2
                Bq, Bk, Bv = T.B("p3q", hb), T.B("p3k", hb), T.B("p3v", hb)
                T.dma("sp", qh[hb][:], qT[h * 128:(h + 1) * 128, :], W=[Bq])
                T.dma("sp", kh[hb][:], kT[h * 128:(h + 1) * 128, :], W=[Bk])
                T.dma("sp", vh[hb][:], vtm.rearrange("(c p) f -> p c f", p=128)[:, :, h * 128:(h + 1) * 128], W=[Bv])
                T.op("dve", lambda: nc.vector.tensor_reduce(out=ksum[:], in_=kh[hb][:].rearrange("p (n l) -> p n l", n=8), axis=AX.X, op=ALU.add),
                     R=[Bk], W=[T.B("ksum")])
                T.op("dve", lambda: nc.vector.tensor_copy(out=ksb[:], in_=ksum[:]), R=[T.B("ksum")], W=[T.B("ksb")])
                for qt in range(8):
                    T.op("pe", lambda: nc.tensor.matmul(bank(7)[:, qt * 8:(qt + 1) * 8], qh[hb][:, qt * 128:(qt + 1) * 128], ksb[:], start=True, stop=True),
                         R=[Bq, T.B("ksb")], W=[pb(7)])
                T.op("dve", lambda: nc.vector.tensor_tensor(out=gate[:], in0=bank(7)[:, 0:64], in1=penG[:], op=ALU.add),
                     R=[pb(7), T.B("penG")], W=[T.B("gate")])
                for qt in range(8):
                    T.op("dve", lambda: nc.vector.max(out=top8[:, qt * 8:(qt + 1) * 8], in_=gate[:, qt * 8:(qt + 1) * 8]),
                         R=[T.B("gate")], W=[T.B("top8")])
                for qt in range(8):
                    T.op("dve", lambda: nc.vector.tensor_scalar(out=sel[:, qt * 8:(qt + 1) * 8], in0=gate[:, qt * 8:(qt + 1) * 8],
                                                                 scalar1=top8[:, qt * 8 + 2:qt * 8 + 3], scalar2=None, op0=ALU.is_ge),
                         R=[T.B("gate"), T.B("top8")], W=[T.B("sel")])
                T.op("dve", lambda: nc.vector.tensor_tensor(out=sel[:], in0=sel[:], in1=own01[:], op=ALU.max),
                     R=[T.B("sel"), T.B("own01")], W=[T.B("sel")])
                T.op("dve", lambda: nc.vector.tensor_scalar(out=sel[:], in0=sel[:], scalar1=30000.0, scalar2=-30000.0, op0=ALU.mult, op1=ALU.add),
                     R=[T.B("sel")], W=[T.B("sel")])
                T.op("dve", lambda: nc.vector.tensor_tensor(out=biasb[:], in0=sel[:], in1=dis[:], op=ALU.add),
                     R=[T.B("sel"), T.B("dis")], W=[T.B("biasb")])

            def geom(qt):
                j, s_ = qt // 2, qt % 2
                return (4 + j) * 256 + (s_ + 1) * 128, 4 + j

            def stage_a(h, qt, sl):
                hb = h % 2
                slope = 2.0 ** (-8.0 / NH * (h + 1))
                Bq, Bk = T.B("p3q", hb), T.B("p3k", hb)
                ncols, nfull = geom(qt)
                c0 = 0
                while c0 < ncols:
                    n = min(512, ncols - c0)
                    bk = c0 // 512
                    T.op("pe", lambda: nc.tensor.matmul(bank(bk)[:, 0:n], qh[hb][:, qt * 128:(qt + 1) * 128], kh[hb][:, c0:c0 + n], start=True, stop=True),
                         R=[Bq, Bk], W=[pb(bk)])
                    c0 += n
                w0 = 896 - qt * 128
                Bss, Brm, Bbq, Bsm, Bpt = T.B("ssb", sl), T.B("rmax", sl), T.B("bq", sl), T.B("sums", sl), T.B("pt", sl)
                T.op("dve", lambda: nc.vector.scalar_tensor_tensor(out=ssb[sl][:, 0:ncols], in0=tb[:, w0:w0 + ncols], scalar=-slope, in1=PS[:, 0:ncols], op0=ALU.mult, op1=ALU.add),
                     R=[T.B("tb"), pb(0), pb(1), pb(2), pb(3)], W=[Bss])
                T.op("dve", lambda: nc.vector.tensor_reduce(out=rmax[sl][:], in_=ssb[sl][:, 0:ncols], axis=AX.X, op=ALU.max),
                     R=[Bss], W=[Brm])
                T.op("dve", lambda: nc.vector.tensor_scalar(out=bq[sl][:], in0=biasb[:, qt * 8:(qt + 1) * 8], scalar1=rmax[sl][:, 0:1], scalar2=None, op0=ALU.subtract),
                     R=[T.B("biasb"), Brm], W=[Bbq])
                T.op("dve", lambda: nc.vector.memset(sums[sl][:], 0.0), W=[Bsm])
                for n_ in range(nfull + 1):
                    cs = n_ * 256
                    ce = min(cs + 256, ncols)
                    T.op("act", lambda: nc.scalar.activation(out=pt[sl][:, cs:ce], in_=ssb[sl][:, cs:ce], func=AF.Exp, bias=bq[sl][:, n_:n_ + 1], accum_out=sums[sl][:, n_:n_ + 1]),
                         R=[Bss, Bbq], W=[Bpt, Bsm])
                T.op("dve", lambda: nc.vector.tensor_reduce(out=rsum[sl][:], in_=sums[sl][:], axis=AX.X, op=ALU.add), R=[Bsm], W=[T.B("rsum", sl)])
                T.op("dve", lambda: nc.vector.reciprocal(out=rsum[sl][:], in_=rsum[sl][:]), R=[T.B("rsum", sl)], W=[T.B("rsum", sl)])

            def stage_b(h, qt, sl):
                hb = h % 2
                Bv, By, Bpt = T.B("p3v", hb), T.B("p3y", hb), T.B("pt", sl)
                ncols, nfull = geom(qt)
                nck = ncols // 128
                for ck in range(nck):
                    bk = 4 + ck // 8
                    pv = bank(bk).bitcast(BF16)
                    T.op("pe", lambda: nc.tensor.transpose(pv[:, (ck % 8) * 128:(ck % 8 + 1) * 128], pt[sl][:, ck * 128:(ck + 1) * 128], ident[:]),
                         R=[Bpt, T.B("ident")], W=[pb(bk)])
                n1 = min(nck, 8)
                copy_op("act", ptT[:, 0:n1, :], bank(4).bitcast(BF16)[:, 0:n1 * 128].rearrange("p (c t) -> p c t", t=128), [pb(4)], [T.B("ptT", 0)])
                if nck > 8:
                    copy_op("dve", ptT[:, 8:nck, :], bank(5).bitcast(BF16)[:, 0:(nck - 8) * 128].rearrange("p (c t) -> p c t", t=128), [pb(5)], [T.B("ptT", 1)])
                for ck in range(nck):
                    T.op("pe", lambda: nc.tensor.matmul(bank(6)[:, 0:128], ptT[:, ck, :], vh[hb][:, ck, :], start=(ck == 0), stop=(ck == nck - 1)),
                         R=[T.B("ptT", 0), T.B("ptT", 1), Bv], W=[pb(6)])
                T.op("act", lambda: nc.scalar.activation(out=on[:], in_=bank(6)[:, 0:128], func=AF.Copy, scale=rsum[sl][:, 0:1]),
                     R=[pb(6), T.B("rsum", sl)], W=[T.B("on")])
                pv6 = bank(6).bitcast(BF16)
                T.op("pe", lambda: nc.tensor.transpose(pv6[:, 512:640], on[:], ident[:]), R=[T.B("on"), T.B("ident")], W=[pb(6)])
                copy_op("dve", yh[hb][:, qt * 128:(qt + 1) * 128], pv6[:, 512:640], [pb(6)], [By])
                if qt == 7:
                    T.dma("sp", yattnT[h * 128:(h + 1) * 128, :], yh[hb][:], R=[By], W=[T.B("yattnT", h)])

            iters = [(h, qt) for h in range(NH) for qt in range(8)]
            head_setup(0)
            stage_a(0, 0, 0)
            for i, (h, qt) in enumerate(iters):
                if i + 1 < len(iters):
                    hx_, qx_ = iters[i + 1]
                    if qx_ == 0:
                        head_setup(hx_)
                    stage_a(hx_, qx_, (i + 1) % 2)
                stage_b(h, qt, i % 2)

        T.barrier()
        if STOP_AFTER <= 3:
            T.final_wait()
            return nc

        TWO_PI = 2.0 * math.pi

        def sincos(ph_, ang, n, sin_out, cos_out, tagp, Bin):
            shp = [128, n]
            ki = sb(f"{tagp}_ki", shp, mybir.dt.int32, ph_)
            kf = sb(f"{tagp}_kf", shp, F32, ph_)
            r = sb(f"{tagp}_r", shp, F32, ph_)
            m = sb(f"{tagp}_m", shp, F32, ph_)
            Bt = T.B(tagp, "tmp")
            for (shift, dst) in ((0.0, sin_out), (math.pi / 2, cos_out)):
                T.op("dve", lambda: nc.vector.tensor_scalar(out=kf[:], in0=ang, scalar1=shift, scalar2=1.0 / TWO_PI, op0=ALU.add, op1=ALU.mult), R=[Bt, Bin], W=[Bt])
                T.op("dve", lambda: nc.vector.tensor_copy(out=ki[:], in_=kf[:]), R=[Bt], W=[Bt])
                T.op("dve", lambda: nc.vector.tensor_copy(out=kf[:], in_=ki[:]), R=[Bt], W=[Bt])
                T.op("dve", lambda: nc.vector.tensor_scalar(out=r[:], in0=ang, scalar1=shift, scalar2=None, op0=ALU.add), R=[Bt, Bin], W=[Bt])
                T.op("dve", lambda: nc.vector.scalar_tensor_tensor(out=r[:], in0=kf[:], scalar=-TWO_PI, in1=r[:], op0=ALU.mult, op1=ALU.add), R=[Bt], W=[Bt])
                T.op("dve", lambda: nc.vector.tensor_scalar(out=m[:], in0=r[:], scalar1=math.pi, scalar2=-TWO_PI, op0=ALU.is_gt, op1=ALU.mult), R=[Bt], W=[Bt])
                T.op("dve", lambda: nc.vector.tensor_tensor(out=r[:], in0=r[:], in1=m[:], op=ALU.add), R=[Bt], W=[Bt])
                T.op("dve", lambda: nc.vector.tensor_scalar(out=m[:], in0=r[:], scalar1=-math.pi, scalar2=TWO_PI, op0=ALU.is_lt, op1=ALU.mult), R=[Bt], W=[Bt])
                T.op("dve", lambda: nc.vector.tensor_tensor(out=r[:], in0=r[:], in1=m[:], op=ALU.add), R=[Bt], W=[Bt])
                T.op("dve", lambda: nc.vector.tensor_scalar(out=r[:], in0=r[:], scalar1=3.1415925, scalar2=-3.1415925, op0=ALU.min, op1=ALU.max), R=[Bt], W=[Bt])
                T.op("act", lambda: nc.scalar.activation(out=dst, in_=r[:], func=AF.Sin), R=[Bt], W=[Bt, Bin])

        for ph in _phase(4):
            NP_ = 16 * 64
            lr = sb("p4lr", [128, NP_], F32, ph)
            li = sb("p4li", [128, NP_], F32, ph)
            dt_ = sb("p4dt", [128, 16], F32, ph)
            br = sb("p4br", [128, NP_], F32, ph)
            bi = sb("p4bi", [128, NP_], F32, ph)
            BP = T.B("p4prep")
            T.dma("sp", lr[:], s_lr.rearrange("p o q -> p (o q)"), W=[BP])
            T.dma("sp", li[:], s_li.rearrange("p o q -> p (o q)"), W=[BP])
            T.dma("sp", dt_[:], s_ls, W=[BP])
            T.dma("sp", br[:], s_br.rearrange("p o q -> p (o q)"), W=[BP])
            T.dma("sp", bi[:], s_bi.rearrange("p o q -> p (o q)"), W=[BP])
            BB1 = sb("p4BB1", [128, 16, 128], F32, ph)
            BB2 = sb("p4BB2", [128, 16, 128], F32, ph)
            with contextlib.ExitStack() as pp:
                ang = sb("p4ang", [128, NP_], F32, pp)
                mag = sb("p4mag", [128, NP_], F32, pp)
                sn = sb("p4sn", [128, NP_], F32, pp)
                cs = sb("p4cs", [128, NP_], F32, pp)
                ar = sb("p4ar", [128, NP_], F32, pp)
                ai = sb("p4ai", [128, NP_], F32, pp)
                den = sb("p4den", [128, NP_], F32, pp)
                fr = sb("p4fr", [128, NP_], F32, pp)
                fi = sb("p4fi", [128, NP_], F32, pp)
                t1_ = sb("p4t1", [128, NP_], F32, pp)

                def v3(t):
                    return t[:].rearrange("p (o q) -> p o q", o=16)
                dtb = dt_[:].unsqueeze(2).broadcast_to([128, 16, 64])
                T.op("act", lambda: nc.scalar.activation(out=dt_[:], in_=dt_[:], func=AF.Exp), R=[BP], W=[BP])
                T.op("dve", lambda: nc.vector.tensor_tensor(out=v3(ang), in0=v3(li), in1=dtb, op=ALU.mult), R=[BP], W=[BP])
                T.op("dve", lambda: nc.vector.tensor_tensor(out=v3(mag), in0=v3(lr), in1=dtb, op=ALU.mult), R=[BP], W=[BP])
                T.op("act", lambda: nc.scalar.activation(out=mag[:], in_=mag[:], func=AF.Exp), R=[BP], W=[BP])
                sincos(pp, ang[:], NP_, sn[:], cs[:], "p4sc", BP)
                T.B("p4sc", "tmp")
                T.barrier()
                TT = lambda o, a, b, op: T.op("dve", lambda: nc.vector.tensor_tensor(out=o[:], in0=a[:], in1=b[:], op=op), R=[BP], W=[BP])
                TT(ar, mag, cs, ALU.mult)
                TT(ai, mag, sn, ALU.mult)
                TT(den, lr, lr, ALU.mult)
                TT(t1_, li, li, ALU.mult)
                TT(den, den, t1_, ALU.add)
                T.op("dve", lambda: nc.vector.reciprocal(out=den[:], in_=den[:]), R=[BP], W=[BP])
                T.op("dve", lambda: nc.vector.tensor_scalar(out=ar[:], in0=ar[:], scalar1=-1.0, scalar2=None, op0=ALU.add), R=[BP], W=[BP])
                TT(fr, ar, lr, ALU.mult)
                TT(t1_, ai, li, ALU.mult)
                TT(fr, fr, t1_, ALU.add)
                TT(fr, fr, den, ALU.mult)
                TT(fi, ai, lr, ALU.mult)
                TT(t1_, ar, li, ALU.mult)
                TT(fi, fi, t1_, ALU.subtract)
                TT(fi, fi, den, ALU.mult)
                TT(t1_, fr, br, ALU.mult)
                TT(den, fi, bi, ALU.mult)
                TT(t1_, t1_, den, ALU.subtract)
                TT(ang, fr, bi, ALU.mult)
                TT(den, fi, br, ALU.mult)
                TT(ang, ang, den, ALU.add)
                for (dstt, c0_, src) in ((BB1, 0, t1_), (BB1, 64, ang), (BB2, 0, ang), (BB2, 64, t1_)):
                    T.op("dve", lambda: nc.vector.tensor_copy(out=dstt[:, :, c0_:c0_ + 64], in_=v3(src)), R=[BP], W=[BP])
            M1 = sb("p4M1", [128, G * 16], BF16, ph)
            M2 = sb("p4M2", [128, G * 16], BF16, ph)
            with contextlib.ExitStack() as pp:
                m1f = sb("p4m1f", [128, G * 16], F32, pp)
                m2f = sb("p4m2f", [128, G * 16], F32, pp)
                T.dma("sp", m1f[0:64, :], s_crT, W=[BP])
                T.dma("sp", m1f[64:128, :], s_ciT, W=[BP])
                T.dma("sp", m2f[0:64, :], s_ciT, W=[BP])
                T.dma("sp", m2f[64:128, :], s_crT, W=[BP])
                T.op("dve", lambda: nc.vector.tensor_copy(out=M1[0:64, :], in_=m1f[0:64, :]), R=[BP], W=[BP])
                T.op("dve", lambda: nc.vector.tensor_scalar(out=M1[64:128, :], in0=m1f[64:128, :], scalar1=-1.0, scalar2=None, op0=ALU.mult), R=[BP], W=[BP])
                T.op("dve", lambda: nc.vector.tensor_scalar(out=M2[0:64, :], in0=m2f[0:64, :], scalar1=-1.0, scalar2=None, op0=ALU.mult), R=[BP], W=[BP])
                T.op("dve", lambda: nc.vector.tensor_copy(out=M2[64:128, :], in_=m2f[64:128, :]), R=[BP], W=[BP])
            th = sb("p4th", [128, G], F32, ph)
            rho = sb("p4rho", [128, G], F32, ph)
            dts = sb("p4dts", [128, G], F32, ph)
            sgn = sb("p4sgn", [128, 1], F32, ph)
            rowmask = sb("p4rm", [128, 8], F32, ph)
            e16 = sb("p4e16", [128, 16], F32, ph)
            dsk = sb("p4dsk", [128, 16], F32, ph)
            t0r = sb("p4t0", [128, 64], F32, ph)
            t1r = sb("p4t1r", [128, 32], F32, ph)
            for (d_, s_) in ((th, s_liT), (rho, s_lrT), (dts, s_lsT), (sgn, c_sgn), (rowmask, c_rowmask), (e16, c_e16), (dsk, s_d), (t0r, c_t0), (t1r, c_t1)):
                T.dma("sp", d_[:], s_, W=[BP])
            T.op("act", lambda: nc.scalar.activation(out=dts[:], in_=dts[:], func=AF.Exp), R=[BP], W=[BP])
            T.op("dve", lambda: nc.vector.tensor_tensor(out=th[:], in0=th[:], in1=dts[:], op=ALU.mult), R=[BP], W=[BP])
            T.op("dve", lambda: nc.vector.tensor_scalar(out=th[:], in0=th[:], scalar1=sgn[:, 0:1], scalar2=None, op0=ALU.mult), R=[BP], W=[BP])
            T.op("dve", lambda: nc.vector.tensor_tensor(out=rho[:], in0=rho[:], in1=dts[:], op=ALU.mult), R=[BP], W=[BP])
            T.op("act", lambda: nc.scalar.activation(out=rho[:], in_=rho[:], func=AF.Exp), R=[BP], W=[BP])

            thi = sb("p4thi", [128, G], F32, ph)
            nrt = sb("p4nrt", [128, G], F32, ph)
            one_t = sb("p4one", [128, 1], F32, ph)
            ttab = sb("p4tt", [128, TALL], F32, ph)
            T.dma("sp", ttab[:], c_tt, W=[BP])
            T.op("dve", lambda: nc.vector.memset(one_t[:], 1.0), W=[BP])
            T.op("dve", lambda: nc.vector.tensor_scalar(out=thi[:], in0=th[:], scalar1=1.0 / TWO_PI, scalar2=None, op0=ALU.mult), R=[BP], W=[BP])
            T.op("dve", lambda: nc.vector.reciprocal(out=nrt[:], in_=th[:]), R=[BP], W=[BP])
            T.op("dve", lambda: nc.vector.tensor_scalar(out=nrt[:], in0=nrt[:], scalar1=-TWO_PI, scalar2=None, op0=ALU.mult), R=[BP], W=[BP])
            T.barrier()

            I32 = mybir.dt.int32
            PI_SAFE = 3.1415925
            uo = [sb(f"p4u{i}", [128, TALL], BF16, ph) for i in range(2)]
            ki = sb("p4ki", [128, TALL], I32, ph)
            uu = sb("p4uu", [128, TALL], F32, ph)
            sh = sb("p4sh", [128, TALL], F32, ph)
            Ec = [sb(f"p4Ec{i}", [128, TALL], F32, ph) for i in range(3)]
            Es = [sb(f"p4Es{i}", [128, TALL], F32, ph) for i in range(3)]
            tB = sb("p4tB", [128, TALL], F32, ph)
            Sp = [sb(f"p4Sp{i}", [128, TALL], F32, ph) for i in range(2)]
            Wt = [sb(f"p4W{i}", [128, TALL], F32, ph) for i in range(2)]
            Q1 = [sb(f"p4Q1{i}", [128, TOK], BF16, ph) for i in range(2)]
            Q2 = [sb(f"p4Q2{i}", [128, TOK], BF16, ph) for i in range(2)]
            Bp1 = [sb(f"p4Bp1{i}", [128, 128], BF16, ph) for i in range(2)]
            Bp2 = [sb(f"p4Bp2{i}", [128, 128], BF16, ph) for i in range(2)]
            Dp = [sb(f"p4Dp{i}", [128, 16], BF16, ph) for i in range(2)]
            yst = [sb(f"p4y{i}", [16, TOK], F32, ph) for i in range(2)]

            def ssm_tables(g):
                gb = g % 3
                BE, Bk, Buu, Bsh = T.B("p4E", gb), T.B("p4ki"), T.B("p4uu"), T.B("p4sh")
                T.op("dve", lambda: nc.vector.tensor_scalar(out=ki[:], in0=ttab[:], scalar1=thi[:, g:g + 1], scalar2=None, op0=ALU.mult), R=[], W=[Bk])
                T.op("dve", lambda: nc.vector.scalar_tensor_tensor(out=uu[:], in0=ttab[:], scalar=thi[:, g:g + 1], in1=ki[:], op0=ALU.mult, op1=ALU.subtract), R=[Bk], W=[Buu])
                T.op("act", lambda: nc.scalar.activation(out=Es[gb][:], in_=uu[:], func=AF.Sin, scale=TWO_PI * (1.0 - 1e-6)), R=[Buu], W=[BE])
                T.op("act", lambda: nc.scalar.activation(out=sh[:], in_=uu[:], func=AF.Sin, scale=math.pi * (1.0 - 1e-6)), R=[Buu], W=[Bsh])
                T.op("act", lambda: nc.scalar.activation(out=sh[:], in_=sh[:], func=AF.Square), R=[Bsh], W=[Bsh])
                T.op("act", lambda: nc.scalar.activation(out=Ec[gb][:], in_=sh[:], func=AF.Identity, scale=-2.0, bias=one_t[:, 0:1]), R=[Bsh], W=[BE])

            def ssm_bu(g, ub, Bu, hf):
                gb = g % 2
                BBp = T.B("p4Bp", gb)
                for ordr in range(2):
                    for q2 in range(2):
                        bk = ordr * 2 + q2
                        cc = hf * 1024 + q2 * 512
                        lh = Bp1[gb] if ordr == 0 else Bp2[gb]
                        T.op("pe", lambda: nc.tensor.matmul(bank(bk), lh[:], uo[ub][:, cc:cc + 512], start=True, stop=True), R=[BBp, Bu], W=[pb(bk)])

            def ssm_front_a(g, ub, Bu):
                o, gl, gb = g // 8, g % 8, g % 2
                BBp = T.B("p4Bp", gb)
                T.op("dve", lambda: nc.vector.tensor_scalar(out=Bp1[gb][:], in0=BB1[:, o, :], scalar1=rowmask[:, gl:gl + 1], scalar2=None, op0=ALU.mult), R=[], W=[BBp])
                T.op("dve", lambda: nc.vector.tensor_scalar(out=Bp2[gb][:], in0=BB2[:, o, :], scalar1=rowmask[:, gl:gl + 1], scalar2=None, op0=ALU.mult), R=[], W=[BBp])
                T.op("dve", lambda: nc.vector.tensor_scalar(out=Dp[gb][:], in0=e16[:], scalar1=dsk[:, o:o + 1], scalar2=rowmask[:, gl:gl + 1], op0=ALU.mult, op1=ALU.mult), R=[], W=[BBp])
                ssm_bu(g, ub, Bu, 0)

            def ssm_front_b(g, ub, Bu):
                gb = g % 2
                ge = g % 3
                BE, BS = T.B("p4E", ge), T.B("p4Sp", gb)
                for hf in range(2):
                    if hf == 1:
                        ssm_bu(g, ub, Bu, 1)
                    hs = slice(hf * 1024, (hf + 1) * 1024)
                    T.op("dve", lambda: nc.vector.tensor_tensor(out=Sp[gb][:, hs], in0=PS[:, 0:1024], in1=Ec[ge][:, hs], op=ALU.mult), R=[pb(0), pb(1), BE], W=[BS])
                    T.op("dve", lambda: nc.vector.tensor_tensor(out=tB[:, hs], in0=PS[:, 1024:2048], in1=Es[ge][:, hs], op=ALU.mult), R=[pb(2), pb(3), BE], W=[T.B("p4tB")])
                    T.op("pool", lambda: nc.gpsimd.tensor_tensor(out=Sp[gb][:, hs], in0=Sp[gb][:, hs], in1=tB[:, hs], op=ALU.add), R=[T.B("p4tB")], W=[BS])

            def ssm_back(g, ub, Bu):
                gb = g % 2
                ge = g % 3
                BBp, BE, BS, BW, BQ = T.B("p4Bp", gb), T.B("p4E", ge), T.B("p4Sp", gb), T.B("p4W", gb), T.B("p4Q", gb)
                T.op("dve", lambda: nc.vector.tensor_tensor_scan(out=Wt[gb][:], data0=rho[:, g:g + 1].broadcast_to([128, TALL]), data1=Sp[gb][:], initial=0.0, op0=ALU.mult, op1=ALU.add),
                     R=[BS], W=[BW])
                T.op("pool", lambda: nc.gpsimd.tensor_tensor(out=Q1[gb][:], in0=Wt[gb][:, TOK:TALL], in1=Ec[ge][:, TOK:TALL], op=ALU.mult), R=[BW, BE], W=[BQ])
                T.op("pool", lambda: nc.gpsimd.tensor_tensor(out=Q2[gb][:], in0=Wt[gb][:, TOK:TALL], in1=Es[ge][:, TOK:TALL], op=ALU.mult), R=[BW, BE], W=[BQ])
                for q2 in range(2):
                    bk = 4 + q2
                    cs_ = slice(q2 * 512, (q2 + 1) * 512)
                    T.op("pe", lambda: nc.tensor.matmul(bank(bk)[0:16, :], M1[:, g * 16:(g + 1) * 16], Q1[gb][:, cs_], start=True, stop=False), R=[BQ], W=[pb(bk)])
                    T.op("pe", lambda: nc.tensor.matmul(bank(bk)[0:16, :], M2[:, g * 16:(g + 1) * 16], Q2[gb][:, cs_], start=False, stop=False), R=[BQ], W=[pb(bk)])
                    T.op("pe", lambda: nc.tensor.matmul(bank(bk)[0:16, :], Dp[gb][:], uo[ub][:, TOK + q2 * 512:TOK + (q2 + 1) * 512], start=False, stop=True), R=[BBp, Bu], W=[pb(bk)])
                By = T.B("p4y", gb)
                T.op("act", lambda: nc.scalar.activation(out=yst[gb][:], in_=PS[0:16, 2048:3072], func=AF.Copy), R=[pb(4), pb(5)], W=[By])
                T.dma("sp", ypreT[g * 16:(g + 1) * 16, :], yst[gb][:], R=[By], W=[T.B("ypreT", g)])

            ssm_tables(0)
            ssm_tables(1)

            def ssm_u(g):
                ub = (g // 8) % 2
                Bu = T.B("p4u", ub)
                if g % 8 == 0:
                    T.dma("sp", uo[ub][:], uT[(g // 8) * 128:(g // 8 + 1) * 128, :], W=[Bu])
                return ub, Bu

            ub0, Bu0 = ssm_u(0)
            ssm_front_a(0, ub0, Bu0)
            cur = (ub0, Bu0)
            for g in range(G):
                ub, Bu = cur
                ssm_front_b(g, ub, Bu)
                if g + 2 < G:
                    ssm_tables(g + 2)
                if g + 1 < G:
                    nxt = ssm_u(g + 1)
                    ssm_front_a(g + 1, nxt[0], nxt[1])
                ssm_back(g, ub, Bu)
                if g + 1 < G:
                    cur = nxt
        T.barrier()
        for ph in _phase(4):
            yt_ = [sb(f"p4gy{i}", [128, TOK], F32, ph) for i in range(2)]
            g1 = sb("p4g1", [128, TOK], F32, ph)
            g2 = sb("p4g2", [128, TOK], F32, ph)
            go = [sb(f"p4go{i}", [128, TOK], BF16, ph) for i in range(2)]
            for o in range(16):
                b_ = o % 2
                By, Bg, Bo = T.B("p4gy", b_), T.B("p4g"), T.B("p4go", b_)
                T.dma("sp", yt_[b_][:], ypreT[o * 128:(o + 1) * 128, :], W=[By])
                T.op("dve", lambda: nc.vector.tensor_tensor(out=g1[:], in0=yt_[b_][:], in1=yt_[b_][:], op=ALU.mult), R=[By], W=[Bg])
                T.op("dve", lambda: nc.vector.tensor_scalar(out=g1[:], in0=g1[:], scalar1=0.044715, scalar2=1.0, op0=ALU.mult, op1=ALU.add), R=[Bg], W=[Bg])
                T.op("dve", lambda: nc.vector.tensor_tensor(out=g1[:], in0=g1[:], in1=yt_[b_][:], op=ALU.mult), R=[Bg, By], W=[Bg])
                T.op("act", lambda: nc.scalar.activation(out=g2[:], in_=g1[:], func=AF.Sigmoid, scale=2.0 * math.sqrt(2.0 / math.pi)), R=[Bg], W=[T.B("p4g2")])
                T.op("dve", lambda: nc.vector.tensor_tensor(out=go[b_][:], in0=g2[:], in1=yt_[b_][:], op=ALU.mult), R=[T.B("p4g2"), By], W=[Bo])
                T.dma("sp", ygT[o * 128:(o + 1) * 128, :], go[b_][:], R=[Bo], W=[T.B("ygT", o)])
        T.barrier()
        if STOP_AFTER <= 4:
            T.final_wait()
            return nc

        for ph in _phase(5):
            act = sb("p5act", [128, 16, TOK], BF16, ph)
            actb = load_act(act, ygT, 2048, 0, TOK, "p5act")
            wa = [sb(f"p5wa{i}", [128, 16, 512], BF16, ph) for i in range(2)]
            wb_ = [sb(f"p5wb{i}", [128, 16, 512], BF16, ph) for i in range(2)]
            sg = sb("p5sg", [128, 512], F32, ph)
            st = [sb(f"p5st{i}", [128, TOK], BF16, ph) for i in range(2)]
            n_ = 0
            for cg in range(4):
                wal = load_w(wa[cg % 2], w_glu, 2048, cg * 512, 512, ("p5wa", cg % 2))
                wbl = load_w(wb_[cg % 2], w_glu, 2048, 2048 + cg * 512, 512, ("p5wb", cg % 2))
                for nch in range(4):
                    stb = T.B("p5st", n_ % 2)
                    for tt in range(2):
                        bkA, bkB = (n_ * 4 + tt * 2) % 8, (n_ * 4 + tt * 2 + 1) % 8
                        for kc in range(16):
                            T.op("pe", lambda: nc.tensor.matmul(bank(bkA), wa[cg % 2][:, kc, nch * 128:(nch + 1) * 128], act[:, kc, tt * 512:(tt + 1) * 512], start=(kc == 0), stop=(kc == 15)), R=actb + wal, W=[pb(bkA)])
                        for kc in range(16):
                            T.op("pe", lambda: nc.tensor.matmul(bank(bkB), wb_[cg % 2][:, kc, nch * 128:(nch + 1) * 128], act[:, kc, tt * 512:(tt + 1) * 512], start=(kc == 0), stop=(kc == 15)), R=actb + wbl, W=[pb(bkB)])
                        T.op("act", lambda: nc.scalar.activation(out=sg[:], in_=bank(bkB), func=AF.Sigmoid), R=[pb(bkB)], W=[T.B("p5sg")])
                        T.op("dve", lambda: nc.vector.tensor_tensor(out=st[n_ % 2][:, tt * 512:(tt + 1) * 512], in0=bank(bkA), in1=sg[:], op=ALU.mult), R=[pb(bkA), T.B("p5sg")], W=[stb])
                    r0 = cg * 512 + nch * 128
                    T.dma("sp", yssmT[r0:r0 + 128, :], st[n_ % 2][:], R=[stb], W=[T.B("yssmT", r0)])
                    n_ += 1
        T.barrier()
        for ph in _phase(5):
            actA = sb("p5aA", [128, 16, TOK], BF16, ph)
            actS = sb("p5aS", [128, 16, TOK], BF16, ph)
            aAb = load_act(actA, yattnT, 2048, 0, TOK, "p5aA")
            aSb = load_act(actS, yssmT, 2048, 0, TOK, "p5aS")
            wa = [sb(f"p5mwa{i}", [128, 16, 512], BF16, ph) for i in range(2)]
            wb_ = [sb(f"p5mwb{i}", [128, 16, 512], BF16, ph) for i in range(2)]
            ga_t = [sb(f"p5ga{i}", [128, TOK], BF16, ph) for i in range(2)]
            gs_t = [sb(f"p5gs{i}", [128, TOK], BF16, ph) for i in range(2)]
            t1_ = sb("p5mt1", [128, 512], F32, ph)
            t2_ = sb("p5mt2", [128, 512], F32, ph)
            st = [sb(f"p5mst{i}", [128, TOK], BF16, ph) for i in range(2)]
            n_ = 0
            for cg in range(8):
                wal = load_w(wa[cg % 2], w_oa, 2048, cg * 512, 512, ("p5wa", cg % 2))
                wbl = load_w(wb_[cg % 2], w_os, 2048, cg * 512, 512, ("p5wb", cg % 2))
                for nch in range(4):
                    r0 = cg * 512 + nch * 128
                    stb = T.B("p5st", n_ % 2)
                    Bga, Bgs = T.B("p5ga", n_ % 2), T.B("p5gs", n_ % 2)
                    T.dma("sp", ga_t[n_ % 2][:], sga[r0:r0 + 128, :], W=[Bga])
                    T.dma("sp", gs_t[n_ % 2][:], sgs[r0:r0 + 128, :], W=[Bgs])
                    for tt in range(2):
                        bkA, bkB = (n_ * 4 + tt * 2) % 8, (n_ * 4 + tt * 2 + 1) % 8
                        for kc in range(16):
                            T.op("pe", lambda: nc.tensor.matmul(bank(bkA), wa[cg % 2][:, kc, nch * 128:(nch + 1) * 128], actA[:, kc, tt * 512:(tt + 1) * 512], start=(kc == 0), stop=(kc == 15)), R=aAb + wal, W=[pb(bkA)])
                        for kc in range(16):
                            T.op("pe", lambda: nc.tensor.matmul(bank(bkB), wb_[cg % 2][:, kc, nch * 128:(nch + 1) * 128], actS[:, kc, tt * 512:(tt + 1) * 512], start=(kc == 0), stop=(kc == 15)), R=aSb + wbl, W=[pb(bkB)])
                        ts_ = slice(tt * 512, (tt + 1) * 512)
                        T.op("dve", lambda: nc.vector.tensor_tensor(out=t1_[:], in0=bank(bkA), in1=ga_t[n_ % 2][:, ts_], op=ALU.mult), R=[pb(bkA), Bga], W=[T.B("p5mt1")])
                        T.op("dve", lambda: nc.vector.tensor_tensor(out=t2_[:], in0=bank(bkB), in1=gs_t[n_ % 2][:, ts_], op=ALU.mult), R=[pb(bkB), Bgs], W=[T.B("p5mt2")])
                        T.op("pool", lambda: nc.gpsimd.tensor_tensor(out=st[n_ % 2][:, ts_], in0=t1_[:], in1=t2_[:], op=ALU.add), R=[T.B("p5mt1"), T.B("p5mt2")], W=[stb])
                    T.dma("sp", mixedT[r0:r0 + 128, :], st[n_ % 2][:], R=[stb], W=[T.B("mixedT", r0)])
                    n_ += 1
        T.barrier()
        for ph in _phase(5):
            act = sb("p5oact", [128, 32, TOK], BF16, ph)
            actb = load_act(act, mixedT, D, 0, TOK, "p5act")
            wbuf = [sb(f"p5w{i}", [128, 32, 512], BF16, ph) for i in range(2)]
            xr = [sb(f"p5x{i}", [128, 512], F32, ph) for i in range(2)]
            ho = [sb(f"p5h{i}", [128, 512], F32, ph) for i in range(2)]
            n_ = 0
            for cg in range(8):
                wbl = load_w(wbuf[cg % 2], w_out, D, cg * 512, 512, ("p5w", cg % 2))
                for tch in range(8):
                    bk = n_ % 8
                    Bx, Bh = T.B("p5x", n_ % 2), T.B("p5h", n_ % 2)
                    T.dma("sp", xr[n_ % 2][:], xin[TOK + tch * 128:TOK + (tch + 1) * 128, cg * 512:(cg + 1) * 512], W=[Bx])
                    for kc in range(32):
                        T.op("pe", lambda: nc.tensor.matmul(bank(bk), act[:, kc, tch * 128:(tch + 1) * 128], wbuf[cg % 2][:, kc, :], start=(kc == 0), stop=(kc == 31)), R=actb + wbl, W=[pb(bk)])
                    T.op("dve", lambda: nc.vector.tensor_tensor(out=ho[n_ % 2][:], in0=bank(bk), in1=xr[n_ % 2][:], op=ALU.add), R=[pb(bk), Bx], W=[Bh])
                    T.dma("sp", h1[tch * 128:(tch + 1) * 128, cg * 512:(cg + 1) * 512], ho[n_ % 2][:], R=[Bh], W=[T.B("h1", tch, cg)])
                    n_ += 1
        T.barrier()
        if STOP_AFTER <= 5:
            T.final_wait()
            return nc

        for ph in _phase(6):
            selw = sb("p6selw", [128, 8, NSLOT], BF16, ph)
            with contextlib.ExitStack() as p6a:
                gf = sb("p6gf", [128, D], F32, p6a)
                T.dma("sp", gf[:], gffn, W=[T.B("gf")])
                wr = sb("p6wr", [128, 32, 72], BF16, p6a)
                T.dma("pool", wr[:], w_rt.rearrange("(c p) n -> p c n", p=128), W=[T.B("wr")], max_dma_last_dim=4096)
                brt = sb("p6brt", [128, 72], F32, p6a)
                T.dma("sp", brt[:], b_rt, W=[T.B("brt")])
                iot = sb("p6iota", [128, NSLOT], F32, p6a)
                T.dma("sp", iot[:], c_iota, W=[T.B("iota")])
                ecap = sb("p6ecap", [128, NE], F32, p6a)
                T.dma("sp", ecap[:], c_ecap, W=[T.B("ecap")])
                lt = sb("p6lt", [128, 128], BF16, p6a)
                T.dma("sp", lt[:], c_lt, W=[T.B("lt")])
                ones = sb("p6ones", [128, 128], BF16, p6a)
                T.dma("sp", ones[:], c_ones, W=[T.B("ones")])
                ht = [sb(f"p6h{i}", [128, D], F32, p6a) for i in range(2)]
                hnb = [sb(f"p6hn{i}", [128, D], BF16, p6a) for i in range(2)]
                hnT = sb("p6hnT", [128, 32, 128], BF16, p6a)
                ss = sb("p6ss", [128, 1], F32, p6a)
                rs = sb("p6rs", [128, 1], F32, p6a)
                lg = sb("p6lg", [128, 72], F32, p6a)
                m8 = sb("p6m8", [128, 8], F32, p6a)
                eg = sb("p6eg", [128, 8], F32, p6a)
                sumg = sb("p6sumg", [128, 1], F32, p6a)
                ohp = sb("p6ohp", [128, 8], F32, p6a)
                lem = sb("p6lem", [128, 64], F32, p6a)
                t8 = sb("p6t8", [128, 8], F32, p6a)
                dv = sb("p6dv", [128, 1], F32, p6a)
                A1 = sb("p6A1", [128, 8, 64], F32, p6a)
                A2 = sb("p6A2", [128, 8, 64], F32, p6a)
                Ab = sb("p6Ab", [128, 8, 64], BF16, p6a)
                w1 = sb("p6w1", [128, 8], F32, p6a)
                w2 = sb("p6w2", [128, 8], F32, p6a)
                for t in range(8):
                    b_ = t % 2
                    Bh, Bn = T.B("p6h", b_), T.B("p6hn", b_)
                    T.dma("sp", ht[b_][:], h1[t * 128:(t + 1) * 128, :], W=[Bh])
                    rms_rstd(p6a, ht[b_][:], Bh, hnb[b_][:], Bn, ss[:], rs[:], "p6")
                    T.op("dve", lambda: nc.vector.scalar_tensor_tensor(out=hnb[b_][:], in0=ht[b_][:], scalar=rs[:, 0:1], in1=gf[:], op0=ALU.mult, op1=ALU.mult),
                         R=[Bh, T.B("p6", "rs"), T.B("gf")], W=[Bn])
                    T.dma("sp", hn[t * 128:(t + 1) * 128, :], hnb[b_][:], R=[Bn], W=[T.B("hn", t)])
                    for q4 in range(4):
                        bk = q4
                        pview = bank(bk).bitcast(BF16)
                        for c8 in range(8):
                            c = q4 * 8 + c8
                            T.op("pe", lambda: nc.tensor.transpose(pview[:, c8 * 128:(c8 + 1) * 128], hnb[b_][:, c * 128:(c + 1) * 128], ident[:]), R=[Bn, T.B("ident")], W=[pb(bk)])
                        copy_op(evac_engine(), hnT[:, q4 * 8:(q4 + 1) * 8, :], pview.rearrange("p (c t) -> p c t", c=8), [pb(bk)], [T.B("hnT", q4)])
                    for kc in range(32):
                        T.op("pe", lambda: nc.tensor.matmul(bank(4)[:, 0:72], hnT[:, kc, :], wr[:, kc, :], start=(kc == 0), stop=(kc == 31)),
                             R=[T.B("hnT", kc // 8), T.B("wr")], W=[pb(4)])
                    BR = T.B("p6route")
                    T.op("dve", lambda: nc.vector.tensor_tensor(out=lg[:], in0=bank(4)[:, 0:72], in1=brt[:], op=ALU.add), R=[pb(4), T.B("brt")], W=[BR])
                    T.op("dve", lambda: nc.vector.max(out=m8[:], in_=lg[:, 0:8]), R=[BR], W=[BR])
                    T.op("dve", lambda: nc.vector.tensor_scalar(out=eg[:], in0=lg[:, 0:8], scalar1=m8[:, 0:1], scalar2=None, op0=ALU.subtract), R=[BR], W=[BR])
                    T.op("act", lambda: nc.scalar.activation(out=eg[:], in_=eg[:], func=AF.Exp, accum_out=sumg[:]), R=[BR], W=[BR])
                    T.op("dve", lambda: nc.vector.reciprocal(out=sumg[:], in_=sumg[:]), R=[BR], W=[BR])
                    T.op("dve", lambda: nc.vector.tensor_scalar(out=ohp[:], in0=lg[:, 0:8], scalar1=m8[:, 0:1], scalar2=None, op0=ALU.is_equal), R=[BR], W=[BR])
                    T.op("dve", lambda: nc.vector.tensor_scalar(out=ohp[:], in0=ohp[:], scalar1=1e30, scalar2=-1e30, op0=ALU.mult, op1=ALU.add), R=[BR], W=[BR])
                    T.op("dve", lambda: nc.vector.tensor_tensor(out=lem[:].rearrange("p (g e) -> p g e", g=8), in0=lg[:, 8:72].rearrange("p (g e) -> p g e", g=8),
                                                                 in1=ohp[:].unsqueeze(2).broadcast_to([128, 8, 8]), op=ALU.add), R=[BR], W=[BR])
                    T.op("dve", lambda: nc.vector.max(out=t8[:], in_=lem[:]), R=[BR], W=[BR])
                    T.op("dve", lambda: nc.vector.tensor_tensor(out=dv[:], in0=t8[:, 0:1], in1=t8[:, 1:2], op=ALU.subtract), R=[BR], W=[BR])
                    T.op("act", lambda: nc.scalar.activation(out=dv[:], in_=dv[:], func=AF.Sigmoid), R=[BR], W=[BR])
                    T.op("dve", lambda: nc.vector.tensor_tensor(out=w1[:, t:t + 1], in0=dv[:], in1=sumg[:], op=ALU.mult), R=[BR], W=[T.B("p6w")])
                    T.op("dve", lambda: nc.vector.tensor_tensor(out=w2[:, t:t + 1], in0=sumg[:], in1=w1[:, t:t + 1], op=ALU.subtract), R=[BR, T.B("p6w")], W=[T.B("p6w")])
                    T.op("dve", lambda: nc.vector.tensor_scalar(out=A1[:, t, :], in0=lem[:], scalar1=t8[:, 0:1], scalar2=None, op0=ALU.is_equal), R=[BR], W=[T.B("p6A")])
                    T.op("dve", lambda: nc.vector.tensor_scalar(out=A2[:, t, :], in0=lem[:], scalar1=t8[:, 1:2], scalar2=None, op0=ALU.is_equal), R=[BR], W=[T.B("p6A")])
                    T.op("dve", lambda: nc.vector.tensor_tensor(out=Ab[:, t, :], in0=A1[:, t, :], in1=A2[:, t, :], op=ALU.add), R=[T.B("p6A")], W=[T.B("p6Ab")])
                cum = sb("p6cum", [128, 64], F32, p6a)
                tq = sb("p6tq", [128, 64], F32, p6a)
                d1 = sb("p6d1", [128, 1], F32, p6a)
                ok = sb("p6ok", [128, 1], F32, p6a)
                dd = sb("p6dd", [128, 4], F32, p6a)
                tmpS = sb("p6tmpS", [128, NSLOT], BF16, p6a)
                for t in range(8):
                    for tp in range(t + 1):
                        lh = ones if tp < t else lt
                        T.op("pe", lambda: nc.tensor.matmul(bank(5)[:, 0:64], lh[:], Ab[:, tp, :], start=(tp == 0), stop=(tp == t)),
                             R=[T.B("p6Ab"), T.B("ones"), T.B("lt")], W=[pb(5)])
                    BC = T.B("p6cum")
                    T.op("dve", lambda: nc.vector.tensor_copy(out=cum[:], in_=bank(5)[:, 0:64]), R=[pb(5)], W=[BC])
                    for k_, Ak in enumerate((A1, A2)):
                        T.op("dve", lambda: nc.vector.tensor_tensor(out=tq[:], in0=cum[:], in1=ecap[:], op=ALU.add), R=[BC, T.B("ecap")], W=[BC])
                        T.op("dve", lambda: nc.vector.tensor_tensor(out=tq[:], in0=tq[:], in1=Ak[:, t, :], op=ALU.mult), R=[BC, T.B("p6A")], W=[BC])
                        T.op("dve", lambda: nc.vector.tensor_reduce(out=d1[:], in_=tq[:], axis=AX.X, op=ALU.add), R=[BC], W=[BC])
                        T.op("dve", lambda: nc.vector.tensor_scalar(out=tq[:], in0=cum[:], scalar1=float(CAP), scalar2=None, op0=ALU.is_lt), R=[BC], W=[BC])
                        T.op("dve", lambda: nc.vector.tensor_tensor(out=tq[:], in0=tq[:], in1=Ak[:, t, :], op=ALU.mult), R=[BC, T.B("p6A")], W=[BC])
                        T.op("dve", lambda: nc.vector.tensor_reduce(out=ok[:], in_=tq[:], axis=AX.X, op=ALU.add), R=[BC], W=[BC])
                        T.op("dve", lambda: nc.vector.tensor_scalar(out=d1[:], in0=d1[:], scalar1=1.0, scalar2=ok[:, 0:1], op0=ALU.add, op1=ALU.mult), R=[BC], W=[BC])
                        T.op("dve", lambda: nc.vector.tensor_scalar(out=dd[:, k_:k_ + 1], in0=d1[:], scalar1=-1.0, scalar2=None, op0=ALU.add), R=[BC], W=[BC])
                    wk1, wk2 = w1[:, t:t + 1], w2[:, t:t + 1]
                    T.op("dve", lambda: nc.vector.tensor_scalar(out=tmpS[:], in0=iot[:], scalar1=dd[:, 0:1], scalar2=wk1, op0=ALU.is_equal, op1=ALU.mult),
                         R=[BC, T.B("iota"), T.B("p6w")], W=[T.B("tmpS")])
                    T.op("dve", lambda: nc.vector.tensor_scalar(out=selw[:, t, :], in0=iot[:], scalar1=dd[:, 1:2], scalar2=wk2, op0=ALU.is_equal, op1=ALU.mult),
                         R=[BC, T.B("iota"), T.B("p6w")], W=[T.B("selw", t)])
                    T.op("pool", lambda: nc.gpsimd.tensor_tensor(out=selw[:, t, :], in0=selw[:, t, :], in1=tmpS[:], op=ALU.add), R=[T.B("tmpS")], W=[T.B("selw", t)])
            T.barrier()
            with contextlib.ExitStack() as p6c:
                selb = sb("p6selb", [128, 8, NSLOT], BF16, p6c)
                for t in range(8):
                    T.op("dve", lambda: nc.vector.tensor_scalar(out=selb[:, t, :], in0=selw[:, t, :], scalar1=0.0, scalar2=None, op0=ALU.is_gt), R=[T.B("selw", t)], W=[T.B("selb", t)])
                hcg = [sb(f"p6hcg{i}", [128, 8, 512], BF16, p6c) for i in range(2)]
                xst = [sb(f"p6xst{i}", [128, 512], BF16, p6c) for i in range(2)]
                n_ = 0
                for cg in range(8):
                    Bh = T.B("p6hcg", cg % 2)
                    T.dma("sp", hcg[cg % 2][:], hn.rearrange("(t p) f -> p t f", p=128)[:, :, cg * 512:(cg + 1) * 512], W=[Bh])
                    for s in range(32):
                        bk = n_ % 8
                        for t in range(8):
                            T.op("pe", lambda: nc.tensor.matmul(bank(bk), selb[:, t, s * 128:(s + 1) * 128], hcg[cg % 2][:, t, :], start=(t == 0), stop=(t == 7)),
                                 R=[T.B("selb", t), Bh], W=[pb(bk)])
                        Bx = T.B("p6xst", n_ % 2)
                        copy_op(evac_engine(), xst[n_ % 2][:], bank(bk), [pb(bk)], [Bx])
                        T.dma("sp", xs[s * 128:(s + 1) * 128, cg * 512:(cg + 1) * 512], xst[n_ % 2][:], R=[Bx], W=[T.B("xs", s, cg)])
                        n_ += 1
            T.barrier()
            with contextlib.ExitStack() as p6d:
                wg = sb("p6wg", [128, 32, 512], BF16, p6d)
                wu = sb("p6wu", [128, 32, 512], BF16, p6d)
                wd = sb("p6wd", [128, 4, D], BF16, p6d)
                xrow = [sb(f"p6xr{i}", [128, D], BF16, p6d) for i in range(2)]
                xT = [sb(f"p6xT{i}", [128, 32, 128], BF16, p6d) for i in range(2)]
                sgt = sb("p6sg", [64, 512], F32, p6d)
                hd = sb("p6hd", [64, 512], BF16, p6d)
                hT = sb("p6hT", [128, 4, 64], BF16, p6d)
                yst = [sb(f"p6ys{i}", [64, 2048], BF16, p6d) for i in range(2)]
                ny = 0
                for s in range(32):
                    sb_ = s % 2
                    Bxr = T.B("p6xr", sb_)
                    T.dma("sp", xrow[sb_][:], xs[s * 128:(s + 1) * 128, :], W=[Bxr])
                    for q4 in range(4):
                        bk = 4 + q4 % 2
                        pview = bank(bk).bitcast(BF16)
                        for c8 in range(8):
                            c = q4 * 8 + c8
                            T.op("pe", lambda: nc.tensor.transpose(pview[:, c8 * 128:(c8 + 1) * 128], xrow[sb_][:, c * 128:(c + 1) * 128], ident[:]), R=[Bxr, T.B("ident")], W=[pb(bk)])
                        copy_op(evac_engine(), xT[sb_][:, q4 * 8:(q4 + 1) * 8, :], pview.rearrange("p (c t) -> p c t", c=8), [pb(bk)], [T.B("p6xT", sb_, q4)])
                    BxT = [T.B("p6xT", sb_, q4) for q4 in range(4)]
                    for hf in range(2):
                        e = 2 * s + hf
                        wgl = load_w(wg, w_gate[e], D, 0, DE, ("p6wg",))
                        wul = load_w(wu, w_up[e], D, 0, DE, ("p6wu",))
                        wdl = load_w(wd, w_down[e], DE, 0, D, ("p6wd",))
                        for kc in range(32):
                            T.op("pe", lambda: nc.tensor.matmul(bank(0)[0:64, :], xT[sb_][:, kc, hf * 64:(hf + 1) * 64], wg[:, kc, :], start=(kc == 0), stop=(kc == 31)), R=BxT + [wgl[kc // 8]], W=[pb(0)])
                        for kc in range(32):
                            T.op("pe", lambda: nc.tensor.matmul(bank(1)[0:64, :], xT[sb_][:, kc, hf * 64:(hf + 1) * 64], wu[:, kc, :], start=(kc == 0), stop=(kc == 31)), R=BxT + [wul[kc // 8]], W=[pb(1)])
                        T.op("act", lambda: nc.scalar.activation(out=sgt[:], in_=bank(0)[0:64, :], func=AF.Silu), R=[pb(0)], W=[T.B("p6sg")])
                        T.op("dve", lambda: nc.vector.tensor_tensor(out=hd[:], in0=bank(1)[0:64, :], in1=sgt[:], op=ALU.mult), R=[pb(1), T.B("p6sg")], W=[T.B("p6hd")])
                        pv = bank(6).bitcast(BF16)
                        for kc in range(4):
                            T.op("pe", lambda: nc.tensor.transpose(pv[:, kc * 64:(kc + 1) * 64], hd[:, kc * 128:(kc + 1) * 128], ident[0:64, 0:64]), R=[T.B("p6hd"), T.B("ident")], W=[pb(6)])
                        copy_op("act", hT[:], pv[:, 0:256].rearrange("p (c t) -> p c t", c=4), [pb(6)], [T.B("p6hT")])
                        for half in range(2):
                            By = T.B("p6ys", ny % 2)
                            for c4 in range(4):
                                cgi = half * 4 + c4
                                bk = 2 + (cgi % 2)
                                for kc in range(4):
                                    T.op("pe", lambda: nc.tensor.matmul(bank(bk)[0:64, :], hT[:, kc, :], wd[:, kc, cgi * 512:(cgi + 1) * 512], start=(kc == 0), stop=(kc == 3)), R=[T.B("p6hT"), wdl[kc]], W=[pb(bk)])
                                copy_op(evac_engine(), yst[ny % 2][:, c4 * 512:(c4 + 1) * 512], bank(bk)[0:64, :], [pb(bk)], [By])
                            T.dma("sp", ys[e * 64:(e + 1) * 64, half * 2048:(half + 1) * 2048], yst[ny % 2][:], R=[By], W=[T.B("ys", e, half)])
                            ny += 1
            T.barrier()
            with contextlib.ExitStack() as p6e:
                swT = sb("p6swT", [128, 32, 8, 128], BF16, p6e)
                for s in range(32):
                    bk = s % 2
                    pv = bank(bk).bitcast(BF16)
                    for t in range(8):
                        T.op("pe", lambda: nc.tensor.transpose(pv[:, t * 128:(t + 1) * 128], selw[:, t, s * 128:(s + 1) * 128], ident[:]), R=[T.B("selw", t), T.B("ident")], W=[pb(bk)])
                    copy_op(evac_engine(), swT[:, s, :, :], pv.rearrange("p (t k) -> p t k", t=8), [pb(bk)], [T.B("swT", s)])
                ycg = [sb(f"p6ycg{i}", [128, 32, 512], BF16, p6e) for i in range(2)]
                hr = [sb(f"p6hr{i}", [128, 512], F32, p6e) for i in range(2)]
                ho = [sb(f"p6ho{i}", [128, 512], F32, p6e) for i in range(2)]
                n_ = 0
                swl = [T.B("swT", s) for s in range(32)]
                for cg in range(8):
                    Byc = T.B("p6ycg", cg % 2)
                    T.dma("sp", ycg[cg % 2][:], ys.rearrange("(s p) f -> p s f", p=128)[:, :, cg * 512:(cg + 1) * 512], W=[Byc])
                    for t in range(8):
                        bk = 2 + n_ % 6
                        Bhr, Bho = T.B("p6hr", n_ % 2), T.B("p6ho", n_ % 2)
                        T.dma("sp", hr[n_ % 2][:], h1[t * 128:(t + 1) * 128, cg * 512:(cg + 1) * 512], W=[Bhr])
                        for s in range(32):
                            T.op("pe", lambda: nc.tensor.matmul(bank(bk), swT[:, s, t, :], ycg[cg % 2][:, s, :], start=(s == 0), stop=(s == 31)), R=swl + [Byc], W=[pb(bk)])
                        T.op("dve", lambda: nc.vector.tensor_tensor(out=ho[n_ % 2][:], in0=bank(bk), in1=hr[n_ % 2][:], op=ALU.add), R=[pb(bk), Bhr], W=[Bho])
                        T.dma("sp", h2[t * 128:(t + 1) * 128, cg * 512:(cg + 1) * 512], ho[n_ % 2][:], R=[Bho], W=[T.B("h2", t, cg)])
                        n_ += 1
        T.barrier()
        for ph in _phase(7):
            gfi = sb("p7g", [128, D], F32, ph)
            T.dma("sp", gfi[:], gfin, W=[T.B("gfi")])
            ht = [sb(f"p7h{i}", [128, D], F32, ph) for i in range(2)]
            ot = [sb(f"p7o{i}", [128, D], F32, ph) for i in range(2)]
            junk = sb("p7junk", [128, D], BF16, ph)
            ss = sb("p7ss", [128, 1], F32, ph)
            rs = sb("p7rs", [128, 1], F32, ph)
            for t in range(8):
                b_ = t % 2
                Bh, Bo = T.B("p7h", b_), T.B("p7o", b_)
                T.dma("sp", ht[b_][:], h2[t * 128:(t + 1) * 128, :], W=[Bh])
                rms_rstd(ph, ht[b_][:], Bh, junk[:], T.B("p7junk"), ss[:], rs[:], "p7")
                T.op("dve", lambda: nc.vector.scalar_tensor_tensor(out=ot[b_][:], in0=ht[b_][:], scalar=rs[:, 0:1], in1=gfi[:], op0=ALU.mult, op1=ALU.mult),
                     R=[Bh, T.B("p7", "rs"), T.B("gfi")], W=[Bo])
                T.dma("sp", out[t * 128:(t + 1) * 128, :], ot[b_][:], R=[Bo], W=[T.B("out", t)])
        T.final_wait()
    return nc


def _consts(half):
    bf = ml_dtypes.bfloat16
    c = {}
    c["c_ident"] = np.eye(128, dtype=np.float32).astype(bf)
    p = np.arange(128)[:, None]
    i = np.arange(2048)[None, :]
    tbv = (p + 1920 - i).astype(np.float32)
    tbv[tbv < 0] = 1.0e6
    c["c_tb"] = tbv
    c["c_lt"] = (np.arange(128)[:, None] < np.arange(128)[None, :]).astype(np.float32).astype(bf)
    c["c_ones"] = np.ones((128, 128), np.float32).astype(bf)
    c["c_iota"] = np.broadcast_to(np.arange(NSLOT, dtype=np.float32)[None, :], (128, NSLOT)).copy()
    c["c_ecap"] = np.broadcast_to((np.arange(NE, dtype=np.float32) * CAP)[None, :], (128, NE)).copy()
    rm = np.zeros((128, 8), np.float32)
    for gl in range(8):
        rm[gl * 16:(gl + 1) * 16, gl] = 1.0
    c["c_rowmask"] = rm
    c["c_e16"] = np.tile(np.eye(16, dtype=np.float32), (8, 1))
    sg = np.ones((128, 1), np.float32)
    sg[64:] = -1.0
    c["c_sgn"] = sg
    c["c_t0"] = np.broadcast_to(np.arange(64, dtype=np.float32)[None, :], (128, 64)).copy()
    c["c_t1"] = np.broadcast_to((64.0 * np.arange(32, dtype=np.float32))[None, :], (128, 32)).copy()
    c["c_tt"] = np.broadcast_to(np.arange(TALL, dtype=np.float32)[None, :], (128, TALL)).copy()
    penG = np.zeros((8, 8), np.float32)
    own = np.zeros((8, 8), np.float32)
    dis = np.zeros((8, 8), np.float32)
    for qt in range(8):
        j = qt // 2
        for n in range(8):
            if n < 4:
                allowed = (half == 1)
            else:
                allowed = (n - 4) < j
            if n == 4 + j:
                own[qt, n] = 1.0
                penG[qt, n] = -1e30
            elif not allowed:
                penG[qt, n] = -1e30
                dis[qt, n] = -30000.0
    c["c_penG"] = np.broadcast_to(penG.reshape(1, 64), (128, 64)).copy()
    c["c_own"] = np.broadcast_to(own.reshape(1, 64), (128, 64)).copy()
    c["c_dis"] = np.broadcast_to(dis.reshape(1, 64), (128, 64)).copy()
    return c


def _shared_inputs(inp):
    f = lambda a: np.ascontiguousarray(np.asarray(a, dtype=np.float32))
    s = {}
    s["gmixT"] = f(np.asarray(inp["g_mix"])[0].reshape(32, 128).T)
    s["w_in"] = f(inp["w_in"][0])
    s["w_glu"] = f(inp["w_glu"][0])
    s["w_o_attn"] = f(inp["w_o_attn"][0])
    s["w_o_ssm"] = f(inp["w_o_ssm"][0])
    s["w_out"] = f(inp["w_out"][0])
    s["gffn"] = f(np.broadcast_to(np.asarray(inp["g_ffn"])[0][None, :], (128, D)))
    s["gfin"] = f(np.broadcast_to(np.asarray(inp["g_final"])[None, :], (128, D)))
    s["w_rt"] = f(np.concatenate([np.asarray(inp["w_router_grp"])[0], np.asarray(inp["w_router_exp"])[0]], axis=1))
    brt = np.concatenate([np.asarray(inp["b_router_grp"])[0], np.asarray(inp["b_router_exp"])[0]])
    s["b_rt"] = f(np.broadcast_to(brt[None, :], (128, 72)))
    s["w_gate"] = f(inp["w_gate"][0])
    s["w_up"] = f(inp["w_up"][0])
    s["w_down"] = f(inp["w_down"][0])
    lam_re = np.asarray(inp["ssm_lam_re"])[0]
    lam_im = np.asarray(inp["ssm_lam_im"])[0]
    ls = np.asarray(inp["ssm_log_step"])[0]
    b_re = np.asarray(inp["ssm_b_re"])[0]
    b_im = np.asarray(inp["ssm_b_im"])[0]
    c_re = np.asarray(inp["ssm_c_re"])[0]
    c_im = np.asarray(inp["ssm_c_im"])[0]
    dsk = np.asarray(inp["ssm_d"])[0]

    def oct_rep(a):
        return f(np.repeat(a.reshape(16, 8, 1, 64), 16, axis=2).transpose(1, 2, 0, 3).reshape(128, 16, 64))
    s["s_lr"] = oct_rep(lam_re)
    s["s_li"] = oct_rep(lam_im)
    s["s_ls"] = f(np.repeat(ls.reshape(16, 8, 1), 16, axis=2).transpose(1, 2, 0).reshape(128, 16))
    s["s_br"] = f(b_re.reshape(16, 8, 64, 16).transpose(1, 3, 0, 2).reshape(128, 16, 64))
    s["s_bi"] = f(b_im.reshape(16, 8, 64, 16).transpose(1, 3, 0, 2).reshape(128, 16, 64))
    s["s_d"] = f(dsk.reshape(16, 8, 16).transpose(1, 2, 0).reshape(128, 16))
    s["s_crT"] = f(c_re.transpose(2, 0, 1).reshape(64, G * 16))
    s["s_ciT"] = f(c_im.transpose(2, 0, 1).reshape(64, G * 16))
    s["s_liT"] = f(np.concatenate([lam_im.T, lam_im.T], axis=0))
    s["s_lrT"] = f(np.concatenate([lam_re.T, lam_re.T], axis=0))
    s["s_lsT"] = f(np.broadcast_to(ls[None, :], (128, G)))
    return s


def make_in_maps(inp, cores=range(8)):
    shared = _shared_inputs(inp)
    x = np.asarray(inp["x"], dtype=np.float32)
    cs = [_consts(0), _consts(1)]
    maps = []
    for core in cores:
        b, half = core // 2, core % 2
        m = dict(shared)
        m.update(cs[half])
        if half == 0:
            xin = np.concatenate([np.zeros((TOK, D), np.float32), x[b, 0:TOK]], axis=0)
        else:
            xin = x[b]
        m["xin"] = np.ascontiguousarray(xin)
        maps.append(m)
    return maps


def kernel(**inputs):
    nc = build()
    in_maps = make_in_maps(inputs)
    res = run_bass_kernel_spmd(nc, in_maps, core_ids=list(range(8)))
    outp = np.zeros((4, 2048, D), np.float32)
    for core in range(8):
        b, half = core // 2, core % 2
        outp[b, half * TOK:(half + 1) * TOK, :] = res.results[core]["out"]
    return outp
```

```python
import contextlib
import math
import numpy as np
import ml_dtypes
import concourse.bass as bass
import concourse.mybir as mybir
from concourse.bass_utils import run_bass_kernel_spmd

F32 = mybir.dt.float32
BF16 = mybir.dt.bfloat16
AF = mybir.ActivationFunctionType
ALU = mybir.AluOpType
AX = mybir.AxisListType

D = 4096
TOK = 1024
TALL = 2048
NH = 16
DH = 128
G = 128
NE = 64
CAP = 64
NSLOT = NE * CAP
DE = 512
NDS = 20
NDS_SW = 8
SEM_LIMIT = 50000
SAME_ENGINE_SYNC = True
STOP_AFTER = 99
PHASES = {1, 2, 3, 4, 5, 6, 7}
INPUT_NAMES = ()


def _phase(n):
    if n in PHASES:
        with contextlib.ExitStack() as st:
            yield st


class Buf:
    __slots__ = ("w", "r")

    def __init__(self):
        self.w = None
        self.r = {}


class Tr:
    def __init__(self, nc, es):
        self.nc = nc
        self.es = es
        self.E = {"pe": nc.tensor, "act": nc.scalar, "dve": nc.vector, "pool": nc.gpsimd, "sp": nc.sync}
        self.sem = {}
        self.cnt = {}
        self.nsem = 0
        self.seen = {e: {} for e in self.E}
        for e in self.E:
            self._newsem(e)
        self.dsems = [es.enter_context(nc.semaphore(f"dma{i}")) for i in range(NDS + NDS_SW)]
        self.dcnt = [0] * (NDS + NDS_SW)
        self.dnext = 0
        self.dnext_sw = 0
        self.bufs = {}
        self.all_events = {}

    def _newsem(self, e):
        self.sem[e] = self.es.enter_context(self.nc.semaphore(f"s_{e}_{self.nsem}"))
        self.nsem += 1
        self.cnt[e] = 0

    def B(self, *key):
        b = self.bufs.get(key)
        if b is None:
            b = Buf()
            self.bufs[key] = b
        return b

    def _deps(self, R, W):
        evs = []
        for b in R:
            if b.w is not None:
                evs.append((b.w, True))
        for b in W:
            if b.w is not None:
                evs.append((b.w, False))
            evs.extend((x, False) for x in b.r.values())
        return evs

    def _wait(self, e, evs):
        need = {}
        for ((sem, val, eng), is_raw) in evs:
            if eng == e and (e == "pe" or e == "sp" or not SAME_ENGINE_SYNC or not is_raw):
                continue
            k = id(sem)
            if k not in need or need[k][1] < val:
                need[k] = (sem, val, eng)
        for k, (sem, val, eng) in need.items():
            if self.seen[e].get(k, 0) >= val:
                continue
            self.E[e].wait_ge(sem, val)
            self.seen[e][k] = val

    def _record(self, ev, R, W):
        k = id(ev[0])
        for b in R:
            b.r[k] = ev
        for b in W:
            b.w = ev
            b.r = {}
        self.all_events[k] = ev

    def op(self, e, fn, R=(), W=()):
        self._wait(e, self._deps(R, W))
        ins = fn()
        self.cnt[e] += 1
        ins.then_inc(self.sem[e], 1)
        ev = (self.sem[e], self.cnt[e], e)
        self._record(ev, R, W)
        if self.cnt[e] >= SEM_LIMIT:
            self._newsem(e)
        return ev

    def dma(self, q, out, in_, R=(), W=(), **kw):
        if q == "pool":
            i = NDS + self.dnext_sw
            self.dnext_sw = (self.dnext_sw + 1) % NDS_SW
        else:
            i = self.dnext
            self.dnext = (i + 1) % NDS
        sem = self.dsems[i]
        evs = self._deps(R, W)
        if self.dcnt[i] > 0:
            evs.append(((sem, self.dcnt[i], "dma"), True))
        self._wait(q, evs)
        self.E[q].dma_start(out=out, in_=in_, **kw).then_inc(sem, 16)
        self.dcnt[i] += 16
        ev = (sem, self.dcnt[i], "dma")
        self._record(ev, R, W)
        return ev

    def barrier(self):
        evs = [(x, True) for x in self.all_events.values()]
        for e in self.E:
            self._wait(e, evs)
        for b in self.bufs.values():
            b.w = None
            b.r = {}

    def final_wait(self):
        evs = [(x, True) for x in self.all_events.values()]
        self._wait("sp", evs)


def build(debug_names=()):
    nc = bass.Bass("TRN2", target_bir_lowering=False)

    def din(name, shape, dt=F32):
        return nc.dram_tensor(name, list(shape), dt, kind="ExternalInput").ap()

    def dscr(name, shape, dt):
        kind = "ExternalOutput" if name in debug_names else ("ExternalInput" if name in INPUT_NAMES else "Internal")
        return nc.dram_tensor(name, list(shape), dt, kind=kind).ap()

    xin = din("xin", [TALL, D])
    gmixT = din("gmixT", [128, 32])
    w_in = din("w_in", [D, 16384])
    w_glu = din("w_glu", [2048, 4096])
    w_oa = din("w_o_attn", [2048, 4096])
    w_os = din("w_o_ssm", [2048, 4096])
    w_out = din("w_out", [D, D])
    gffn = din("gffn", [128, D])
    gfin = din("gfin", [128, D])
    w_rt = din("w_rt", [D, 72])
    b_rt = din("b_rt", [128, 72])
    w_gate = din("w_gate", [NE, D, DE])
    w_up = din("w_up", [NE, D, DE])
    w_down = din("w_down", [NE, DE, D])
    s_lr = din("s_lr", [128, 16, 64])
    s_li = din("s_li", [128, 16, 64])
    s_ls = din("s_ls", [128, 16])
    s_br = din("s_br", [128, 16, 64])
    s_bi = din("s_bi", [128, 16, 64])
    s_d = din("s_d", [128, 16])
    s_crT = din("s_crT", [64, G * 16])
    s_ciT = din("s_ciT", [64, G * 16])
    s_liT = din("s_liT", [128, G])
    s_lrT = din("s_lrT", [128, G])
    s_lsT = din("s_lsT", [128, G])
    c_ident = din("c_ident", [128, 128], BF16)
    c_tb = din("c_tb", [128, 2048])
    c_lt = din("c_lt", [128, 128], BF16)
    c_ones = din("c_ones", [128, 128], BF16)
    c_iota = din("c_iota", [128, NSLOT])
    c_ecap = din("c_ecap", [128, NE])
    c_rowmask = din("c_rowmask", [128, 8])
    c_e16 = din("c_e16", [128, 16])
    c_sgn = din("c_sgn", [128, 1])
    c_t0 = din("c_t0", [128, 64])
    c_t1 = din("c_t1", [128, 32])
    c_tt = din("c_tt", [128, TALL])
    c_penG = din("c_penG", [128, 64])
    c_own = din("c_own", [128, 64])
    c_dis = din("c_dis", [128, 64])

    out = nc.dram_tensor("out", [TOK, D], F32, kind="ExternalOutput").ap()

    aT = dscr("aT", [D, TALL], BF16)
    qT = dscr("qT", [2048, TOK], BF16)
    kT = dscr("kT", [2048, TALL], BF16)
    vtm = dscr("vtm", [TALL, 2048], BF16)
    uT = dscr("uT", [2048, TALL], BF16)
    sga = dscr("sga", [D, TOK], BF16)
    sgs = dscr("sgs", [D, TOK], BF16)
    yattnT = dscr("yattnT", [2048, TOK], BF16)
    ypreT = dscr("ypreT", [2048, TOK], F32)
    ygT = dscr("ygT", [2048, TOK], BF16)
    yssmT = dscr("yssmT", [2048, TOK], BF16)
    mixedT = dscr("mixedT", [D, TOK], BF16)
    h1 = dscr("h1", [TOK, D], F32)
    hn = dscr("hn", [TOK, D], BF16)
    xs = dscr("xs", [NSLOT, D], BF16)
    ys = dscr("ys", [NSLOT, D], BF16)
    h2 = dscr("h2", [TOK, D], F32)
    rdbg = dscr("rdbg", [TOK, 8], F32)

    es = contextlib.ExitStack()
    with es:
        T = Tr(nc, es)
        PS = es.enter_context(nc.psum_tensor("PS", [128, 4096], F32))

        def bank(i):
            return PS[:, i * 512:(i + 1) * 512]

        def pb(i):
            return T.B("ps", i)

        def sb(name, shape, dt, stack):
            return stack.enter_context(nc.sbuf_tensor(name, list(shape), dt))

        ident = sb("ident", [128, 128], BF16, es)
        T.dma("sp", ident[:], c_ident, W=[T.B("ident")])
        ev_state = {"n": 0}

        def evac_engine():
            ev_state["n"] += 1
            return "act" if ev_state["n"] % 2 else "dve"

        def copy_op(eng, out_ap, in_ap, R, W, scale=None):
            if eng == "act":
                if scale is None:
                    T.op("act", lambda: nc.scalar.activation(out=out_ap, in_=in_ap, func=AF.Copy), R=R, W=W)
                else:
                    T.op("act", lambda: nc.scalar.activation(out=out_ap, in_=in_ap, func=AF.Copy, scale=scale), R=R, W=W)
            else:
                if scale is None:
                    T.op("dve", lambda: nc.vector.tensor_copy(out=out_ap, in_=in_ap), R=R, W=W)
                else:
                    T.op("dve", lambda: nc.vector.tensor_scalar(out=out_ap, in0=in_ap, scalar1=scale, scalar2=None, op0=ALU.mult), R=R, W=W)

        def rms_rstd(st, xt_ap, xbuf, junk, jbuf, ss, rs, tag):
            T.op("act", lambda: nc.scalar.activation(out=junk, in_=xt_ap, func=AF.Square, accum_out=ss),
                 R=[xbuf], W=[jbuf, T.B(tag, "ss")])
            T.op("act", lambda: nc.scalar.activation(out=rs, in_=ss, func=AF.Sqrt, scale=1.0 / D, bias=eps_t[:, 0:1]),
                 R=[T.B(tag, "ss"), T.B("eps")], W=[T.B(tag, "rs")])
            T.op("dve", lambda: nc.vector.reciprocal(out=rs, in_=rs), R=[T.B(tag, "rs")], W=[T.B(tag, "rs")])

        eps_t = sb("eps_t", [128, 1], F32, es)
        T.op("dve", lambda: nc.vector.memset(eps_t[:], 1e-6), W=[T.B("eps")])

        for ph in _phase(1):
            gm = sb("gm", [128, 32], F32, ph)
            T.dma("sp", gm[:], gmixT, W=[T.B("gm")])
            xt = [sb(f"p1x{i}", [128, D], F32, ph) for i in range(2)]
            xn = [sb(f"p1xn{i}", [128, D], BF16, ph) for i in range(2)]
            junk = sb("p1junk", [128, D], BF16, ph)
            ss = sb("p1ss", [128, 1], F32, ph)
            rs = sb("p1rs", [128, 1], F32, ph)
            agrp = [sb(f"p1ag{i}", [128, 32, 512], BF16, ph) for i in range(2)]
            for i in range(16):
                xb = T.B("p1x", i % 2)
                xnb = T.B("p1xn", i % 2)
                agb = T.B("p1ag", (i // 4) % 2)
                ag = agrp[(i // 4) % 2]
                T.dma("sp", xt[i % 2][:], xin[i * 128:(i + 1) * 128, :], W=[xb])
                rms_rstd(ph, xt[i % 2][:], xb, junk[:], T.B("p1junk"), ss[:], rs[:], "p1")
                T.op("dve", lambda: nc.vector.tensor_scalar(out=xn[i % 2][:], in0=xt[i % 2][:], scalar1=rs[:, 0:1], scalar2=None, op0=ALU.mult),
                     R=[xb, T.B("p1", "rs")], W=[xnb])
                for q4 in range(4):
                    bk = (i * 4 + q4) % 8
                    pview = bank(bk).bitcast(BF16)
                    for c8 in range(8):
                        c = q4 * 8 + c8
                        T.op("pe", lambda: nc.tensor.transpose(pview[:, c8 * 128:(c8 + 1) * 128], xn[i % 2][:, c * 128:(c + 1) * 128], ident[:]),
                             R=[xnb, T.B("ident")], W=[pb(bk)])
                    o_ap = ag[:, q4 * 8:(q4 + 1) * 8, (i % 4) * 128:(i % 4 + 1) * 128]
                    i_ap = pview.rearrange("p (c t) -> p c t", c=8)
                    g_ap = gm[:, q4 * 8:(q4 + 1) * 8].unsqueeze(2).broadcast_to([128, 8, 128])
                    T.op("dve", lambda: nc.vector.tensor_tensor(out=o_ap, in0=i_ap, in1=g_ap, op=ALU.mult),
                         R=[pb(bk), T.B("gm")], W=[agb])
                if i % 4 == 3:
                    gi = i // 4
                    T.dma("sp", aT.rearrange("(c p) t -> p c t", p=128)[:, :, gi * 512:(gi + 1) * 512], ag[:], R=[agb], W=[T.B("aT", gi)])
        T.barrier()
        if STOP_AFTER <= 1:
            T.final_wait()
            return nc

        def load_act(dst, dram_fm, K, t0, tn, bufkey):
            kc = K // 128
            half = kc // 2 if kc >= 2 else kc
            v = dram_fm.rearrange("(c p) t -> p c t", p=128)
            T.dma("sp", dst[:, 0:half, :], v[:, 0:half, t0:t0 + tn], W=[T.B(bufkey, 0)])
            if half < kc:
                T.dma("sp", dst[:, half:kc, :], v[:, half:kc, t0:t0 + tn], W=[T.B(bufkey, 1)])
            return [T.B(bufkey, 0), T.B(bufkey, 1)] if half < kc else [T.B(bufkey, 0)]

        def load_w(dst, wdram, K, c0, cn, bufkey):
            kc = K // 128
            v = wdram.rearrange("(c p) n -> p c n", p=128)
            step = max(1, kc // 4)
            bl = []
            for j, k0 in enumerate(range(0, kc, step)):
                T.dma("pool", dst[:, k0:k0 + step, :], v[:, k0:k0 + step, c0:c0 + cn], W=[T.B(bufkey, j)], max_dma_last_dim=4096)
                bl.append(T.B(bufkey, j))
            return bl

        SCALE = DH ** -0.5
        for ph in _phase(2):
            act = sb("p2act", [128, 32, TOK], BF16, ph)
            wbuf = [sb(f"p2w{i}", [128, 32, 512], BF16, ph) for i in range(2)]
            stg = [sb(f"p2s{i}", [128, TOK], BF16, ph) for i in range(2)]
            stv = [sb(f"p2v{i}", [128, 512], BF16, ph) for i in range(2)]
            nst = [0]
            for ps_ in range(2):
                tok0 = TOK if ps_ == 0 else 0
                actb = load_act(act, aT, D, tok0, TOK, "p2act")
                groups = list(range(32)) if ps_ == 0 else list(range(4, 16))
                for gi, cgp in enumerate(groups):
                    wb = wbuf[gi % 2]
                    wbl = load_w(wb, w_in, D, cgp * 512, 512, ("p2w", gi % 2))
                    kind = ["q", "k", "v", "u", "ga", "ga", "gs", "gs"][cgp // 4]
                    if kind == "v":
                        for tch in range(8):
                            bk = nst[0] % 8
                            for kc in range(32):
                                T.op("pe", lambda: nc.tensor.matmul(bank(bk), act[:, kc, tch * 128:(tch + 1) * 128], wb[:, kc, :], start=(kc == 0), stop=(kc == 31)),
                                     R=actb + wbl, W=[pb(bk)])
                            sv = stv[nst[0] % 2]
                            svb = T.B("p2v", nst[0] % 2)
                            copy_op(evac_engine(), sv[:], bank(bk), [pb(bk)], [svb])
                            col = (cgp - 8) * 512
                            T.dma("sp", vtm[tok0 + tch * 128: tok0 + (tch + 1) * 128, col:col + 512], sv[:], R=[svb], W=[T.B("vtm", tok0, tch, cgp)])
                            nst[0] += 1
                    else:
                        for nch in range(4):
                            st = stg[nst[0] % 2]
                            stb = T.B("p2s", nst[0] % 2)
                            for tt in range(2):
                                bk = (nst[0] * 2 + tt) % 8
                                for kc in range(32):
                                    T.op("pe", lambda: nc.tensor.matmul(bank(bk), wb[:, kc, nch * 128:(nch + 1) * 128], act[:, kc, tt * 512:(tt + 1) * 512], start=(kc == 0), stop=(kc == 31)),
                                         R=actb + wbl, W=[pb(bk)])
                                o_ap = st[:, tt * 512:(tt + 1) * 512]
                                if kind in ("ga", "gs"):
                                    T.op("act", lambda: nc.scalar.activation(out=o_ap, in_=bank(bk), func=AF.Sigmoid), R=[pb(bk)], W=[stb])
                                elif kind == "q":
                                    copy_op(evac_engine(), o_ap, bank(bk), [pb(bk)], [stb], scale=SCALE)
                                else:
                                    copy_op(evac_engine(), o_ap, bank(bk), [pb(bk)], [stb])
                            r0 = (cgp % 4) * 512 + nch * 128
                            if kind == "q":
                                dst = qT[r0:r0 + 128, :]
                            elif kind == "k":
                                dst = kT[r0:r0 + 128, tok0:tok0 + TOK]
                            elif kind == "u":
                                dst = uT[r0:r0 + 128, tok0:tok0 + TOK]
                            elif kind == "ga":
                                r0 = (cgp - 16) * 512 + nch * 128
                                dst = sga[r0:r0 + 128, :]
                            else:
                                r0 = (cgp - 24) * 512 + nch * 128
                                dst = sgs[r0:r0 + 128, :]
                            T.dma("sp", dst, st[:], R=[stb], W=[T.B("p2out", kind, cgp, nch, ps_)])
                            nst[0] += 1
        T.barrier()
        if STOP_AFTER <= 2:
            T.final_wait()
            return nc

        for ph in _phase(3):
            tb = sb("p3tb", [128, 2048], F32, ph)
            penG = sb("p3penG", [128, 64], F32, ph)
            own01 = sb("p3own", [128, 64], F32, ph)
            dis = sb("p3dis", [128, 64], F32, ph)
            T.dma("sp", tb[:], c_tb, W=[T.B("tb")])
            T.dma("sp", penG[:], c_penG, W=[T.B("penG")])
            T.dma("sp", own01[:], c_own, W=[T.B("own01")])
            T.dma("sp", dis[:], c_dis, W=[T.B("dis")])
            qh = [sb(f"p3q{i}", [128, TOK], BF16, ph) for i in range(2)]
            kh = [sb(f"p3k{i}", [128, TALL], BF16, ph) for i in range(2)]
            vh = [sb(f"p3v{i}", [128, 16, 128], BF16, ph) for i in range(2)]
            ksum = sb("p3ksum", [128, 8], F32, ph)
            ksb = sb("p3ksb", [128, 8], BF16, ph)
            gate = sb("p3gate", [128, 64], F32, ph)
            top8 = sb("p3top8", [128, 64], F32, ph)
            sel = sb("p3sel", [128, 64], F32, ph)
            biasb = sb("p3bias", [128, 64], F32, ph)
            ssb = [sb(f"p3ssb{i}", [128, 2048], F32, ph) for i in range(2)]
            pt = [sb(f"p3p{i}", [128, 2048], BF16, ph) for i in range(2)]
            ptT = sb("p3pT", [128, 16, 128], BF16, ph)
            rmax = [sb(f"p3rmax{i}", [128, 1], F32, ph) for i in range(2)]
            bq = [sb(f"p3bq{i}", [128, 8], F32, ph) for i in range(2)]
            sums = [sb(f"p3sums{i}", [128, 8], F32, ph) for i in range(2)]
            rsum = [sb(f"p3rsum{i}", [128, 1], F32, ph) for i in range(2)]
            on = sb("p3on", [128, 128], BF16, ph)
            yh = [sb(f"p3y{i}", [128, TOK], BF16, ph) for i in range(2)]

            def head_setup(h):
                hb = h % 2
                Bq, Bk, Bv = T.B("p3q", hb), T.B("p3k", hb), T.B("p3v", hb)
                T.dma("sp", qh[hb][:], qT[h * 128:(h + 1) * 128, :], W=[Bq])
                T.dma("sp", kh[hb][:], kT[h * 128:(h + 1) * 128, :], W=[Bk])
                T.dma("sp", vh[hb][:], vtm.rearrange("(c p) f -> p c f", p=128)[:, :, h * 128:(h + 1) * 128], W=[Bv])
                T.op("dve", lambda: nc.vector.tensor_reduce(out=ksum[:], in_=kh[hb][:].rearrange("p (n l) -> p n l", n=8), axis=AX.X, op=ALU.add),
                     R=[Bk], W=[T.B("ksum")])
                T.op("dve", lambda: nc.vector.tensor_copy(out=ksb[:], in_=ksum[:]), R=[T.B("ksum")], W=[T.B("ksb")])
                for qt in range(8):
                    T.op("pe", lambda: nc.tensor.matmul(bank(7)[:, qt * 8:(qt + 1) * 8], qh[hb][:, qt * 128:(qt + 1) * 128], ksb[:], start=True, stop=True),
                         R=[Bq, T.B("ksb")], W=[pb(7)])
                T.op("dve", lambda: nc.vector.tensor_tensor(out=gate[:], in0=bank(7)[:, 0:64], in1=penG[:], op=ALU.add),
                     R=[pb(7), T.B("penG")], W=[T.B("gate")])
                for qt in range(8):
                    T.op("dve", lambda: nc.vector.max(out=top8[:, qt * 8:(qt + 1) * 8], in_=gate[:, qt * 8:(qt + 1) * 8]),
                         R=[T.B("gate")], W=[T.B("top8")])
                for qt in range(8):
                    T.op("dve", lambda: nc.vector.tensor_scalar(out=sel[:, qt * 8:(qt + 1) * 8], in0=gate[:, qt * 8:(qt + 1) * 8],
                                                                 scalar1=top8[:, qt * 8 + 2:qt * 8 + 3], scalar2=None, op0=ALU.is_ge),
                         R=[T.B("gate"), T.B("top8")], W=[T.B("sel")])
                T.op("dve", lambda: nc.vector.tensor_tensor(out=sel[:], in0=sel[:], in1=own01[:], op=ALU.max),
                     R=[T.B("sel"), T.B("own01")], W=[T.B("sel")])
                T.op("dve", lambda: nc.vector.tensor_scalar(out=sel[:], in0=sel[:], scalar1=30000.0, scalar2=-30000.0, op0=ALU.mult, op1=ALU.add),
                     R=[T.B("sel")], W=[T.B("sel")])
                T.op("dve", lambda: nc.vector.tensor_tensor(out=biasb[:], in0=sel[:], in1=dis[:], op=ALU.add),
                     R=[T.B("sel"), T.B("dis")], W=[T.B("biasb")])

            def geom(qt):
                j, s_ = qt // 2, qt % 2
                return (4 + j) * 256 + (s_ + 1) * 128, 4 + j

            def stage_a(h, qt, sl):
                hb = h % 2
                slope = 2.0 ** (-8.0 / NH * (h + 1))
                Bq, Bk = T.B("p3q", hb), T.B("p3k", hb)
                ncols, nfull = geom(qt)
                c0 = 0
                while c0 < ncols:
                    n = min(512, ncols - c0)
                    bk = c0 // 512
                    T.op("pe", lambda: nc.tensor.matmul(bank(bk)[:, 0:n], qh[hb][:, qt * 128:(qt + 1) * 128], kh[hb][:, c0:c0 + n], start=True, stop=True),
                         R=[Bq, Bk], W=[pb(bk)])
                    c0 += n
                w0 = 896 - qt * 128
                Bss, Brm, Bbq, Bsm, Bpt = T.B("ssb", sl), T.B("rmax", sl), T.B("bq", sl), T.B("sums", sl), T.B("pt", sl)
                T.op("dve", lambda: nc.vector.scalar_tensor_tensor(out=ssb[sl][:, 0:ncols], in0=tb[:, w0:w0 + ncols], scalar=-slope, in1=PS[:, 0:ncols], op0=ALU.mult, op1=ALU.add),
                     R=[T.B("tb"), pb(0), pb(1), pb(2), pb(3)], W=[Bss])
                T.op("dve", lambda: nc.vector.tensor_reduce(out=rmax[sl][:], in_=ssb[sl][:, 0:ncols], axis=AX.X, op=ALU.max),
                     R=[Bss], W=[Brm])
                T.op("dve", lambda: nc.vector.tensor_scalar(out=bq[sl][:], in0=biasb[:, qt * 8:(qt + 1) * 8], scalar1=rmax[sl][:, 0:1], scalar2=None, op0=ALU.subtract),
                     R=[T.B("biasb"), Brm], W=[Bbq])
                T.op("dve", lambda: nc.vector.memset(sums[sl][:], 0.0), W=[Bsm])
                for n_ in range(nfull + 1):
                    cs = n_ * 256
                    ce = min(cs + 256, ncols)
                    T.op("act", lambda: nc.scalar.activation(out=pt[sl][:, cs:ce], in_=ssb[sl][:, cs:ce], func=AF.Exp, bias=bq[sl][:, n_:n_ + 1], accum_out=sums[sl][:, n_:n_ + 1]),
                         R=[Bss, Bbq], W=[Bpt, Bsm])
                T.op("dve", lambda: nc.vector.tensor_reduce(out=rsum[sl][:], in_=sums[sl][:], axis=AX.X, op=ALU.add), R=[Bsm], W=[T.B("rsum", sl)])
                T.op("dve", lambda: nc.vector.reciprocal(out=rsum[sl][:], in_=rsum[sl][:]), R=[T.B("rsum", sl)], W=[T.B("rsum", sl)])

            def stage_b(h, qt, sl):
                hb = h % 2
                Bv, By, Bpt = T.B("p3v", hb), T.B("p3y", hb), T.B("pt", sl)
                ncols, nfull = geom(qt)
                nck = ncols // 128
                for ck in range(nck):
                    bk = 4 + ck // 8
                    pv = bank(bk).bitcast(BF16)
                    T.op("pe", lambda: nc.tensor.transpose(pv[:, (ck % 8) * 128:(ck % 8 + 1) * 128], pt[sl][:, ck * 128:(ck + 1) * 128], ident[:]),
                         R=[Bpt, T.B("ident")], W=[pb(bk)])
                n1 = min(nck, 8)
                copy_op("act", ptT[:, 0:n1, :], bank(4).bitcast(BF16)[:, 0:n1 * 128].rearrange("p (c t) -> p c t", t=128), [pb(4)], [T.B("ptT", 0)])
                if nck > 8:
                    copy_op("dve", ptT[:, 8:nck, :], bank(5).bitcast(BF16)[:, 0:(nck - 8) * 128].rearrange("p (c t) -> p c t", t=128), [pb(5)], [T.B("ptT", 1)])
                for ck in range(nck):
                    T.op("pe", lambda: nc.tensor.matmul(bank(6)[:, 0:128], ptT[:, ck, :], vh[hb][:, ck, :], start=(ck == 0), stop=(ck == nck - 1)),
                         R=[T.B("ptT", 0), T.B("ptT", 1), Bv], W=[pb(6)])
                T.op("act", lambda: nc.scalar.activation(out=on[:], in_=bank(6)[:, 0:128], func=AF.Copy, scale=rsum[sl][:, 0:1]),
                     R=[pb(6), T.B("rsum", sl)], W=[T.B("on")])
                pv6 = bank(6).bitcast(BF16)
                T.op("pe", lambda: nc.tensor.transpose(pv6[:, 512:640], on[:], ident[:]), R=[T.B("on"), T.B("ident")], W=[pb(6)])
                copy_op("dve", yh[hb][:, qt * 128:(qt + 1) * 128], pv6[:, 512:640], [pb(6)], [By])
                if qt == 7:
                    T.dma("sp", yattnT[h * 128:(h + 1) * 128, :], yh[hb][:], R=[By], W=[T.B("yattnT", h)])

            iters = [(h, qt) for h in range(NH) for qt in range(8)]
            head_setup(0)
            stage_a(0, 0, 0)
            for i, (h, qt) in enumerate(iters):
                if i + 1 < len(iters):
                    hx_, qx_ = iters[i + 1]
                    if qx_ == 0:
                        head_setup(hx_)
                    stage_a(hx_, qx_, (i + 1) % 2)
                stage_b(h, qt, i % 2)

        T.barrier()
        if STOP_AFTER <= 3:
            T.final_wait()
            return nc

        TWO_PI = 2.0 * math.pi

        def sincos(ph_, ang, n, sin_out, cos_out, tagp, Bin):
            shp = [128, n]
            ki = sb(f"{tagp}_ki", shp, mybir.dt.int32, ph_)
            kf = sb(f"{tagp}_kf", shp, F32, ph_)
            r = sb(f"{tagp}_r", shp, F32, ph_)
            m = sb(f"{tagp}_m", shp, F32, ph_)
            Bt = T.B(tagp, "tmp")
            for (shift, dst) in ((0.0, sin_out), (math.pi / 2, cos_out)):
                T.op("dve", lambda: nc.vector.tensor_scalar(out=kf[:], in0=ang, scalar1=shift, scalar2=1.0 / TWO_PI, op0=ALU.add, op1=ALU.mult), R=[Bt, Bin], W=[Bt])
                T.op("dve", lambda: nc.vector.tensor_copy(out=ki[:], in_=kf[:]), R=[Bt], W=[Bt])
                T.op("dve", lambda: nc.vector.tensor_copy(out=kf[:], in_=ki[:]), R=[Bt], W=[Bt])
                T.op("dve", lambda: nc.vector.tensor_scalar(out=r[:], in0=ang, scalar1=shift, scalar2=None, op0=ALU.add), R=[Bt, Bin], W=[Bt])
                T.op("dve", lambda: nc.vector.scalar_tensor_tensor(out=r[:], in0=kf[:], scalar=-TWO_PI, in1=r[:], op0=ALU.mult, op1=ALU.add), R=[Bt], W=[Bt])
                T.op("dve", lambda: nc.vector.tensor_scalar(out=m[:], in0=r[:], scalar1=math.pi, scalar2=-TWO_PI, op0=ALU.is_gt, op1=ALU.mult), R=[Bt], W=[Bt])
                T.op("dve", lambda: nc.vector.tensor_tensor(out=r[:], in0=r[:], in1=m[:], op=ALU.add), R=[Bt], W=[Bt])
                T.op("dve", lambda: nc.vector.tensor_scalar(out=m[:], in0=r[:], scalar1=-math.pi, scalar2=TWO_PI, op0=ALU.is_lt, op1=ALU.mult), R=[Bt], W=[Bt])
                T.op("dve", lambda: nc.vector.tensor_tensor(out=r[:], in0=r[:], in1=m[:], op=ALU.add), R=[Bt], W=[Bt])
                T.op("dve", lambda: nc.vector.tensor_scalar(out=r[:], in0=r[:], scalar1=3.1415925, scalar2=-3.1415925, op0=ALU.min, op1=ALU.max), R=[Bt], W=[Bt])
                T.op("act", lambda: nc.scalar.activation(out=dst, in_=r[:], func=AF.Sin), R=[Bt], W=[Bt, Bin])

        for ph in _phase(4):
            NP_ = 16 * 64
            lr = sb("p4lr", [128, NP_], F32, ph)
            li = sb("p4li", [128, NP_], F32, ph)
            dt_ = sb("p4dt", [128, 16], F32, ph)
            br = sb("p4br", [128, NP_], F32, ph)
            bi = sb("p4bi", [128, NP_], F32, ph)
            BP = T.B("p4prep")
            T.dma("sp", lr[:], s_lr.rearrange("p o q -> p (o q)"), W=[BP])
            T.dma("sp", li[:], s_li.rearrange("p o q -> p (o q)"), W=[BP])
            T.dma("sp", dt_[:], s_ls, W=[BP])
            T.dma("sp", br[:], s_br.rearrange("p o q -> p (o q)"), W=[BP])
            T.dma("sp", bi[:], s_bi.rearrange("p o q -> p (o q)"), W=[BP])
            BB1 = sb("p4BB1", [128, 16, 128], F32, ph)
            BB2 = sb("p4BB2", [128, 16, 128], F32, ph)
            with contextlib.ExitStack() as pp:
                ang = sb("p4ang", [128, NP_], F32, pp)
                mag = sb("p4mag", [128, NP_], F32, pp)
                sn = sb("p4sn", [128, NP_], F32, pp)
                cs = sb("p4cs", [128, NP_], F32, pp)
                ar = sb("p4ar", [128, NP_], F32, pp)
                ai = sb("p4ai", [128, NP_], F32, pp)
                den = sb("p4den", [128, NP_], F32, pp)
                fr = sb("p4fr", [128, NP_], F32, pp)
                fi = sb("p4fi", [128, NP_], F32, pp)
                t1_ = sb("p4t1", [128, NP_], F32, pp)

                def v3(t):
                    return t[:].rearrange("p (o q) -> p o q", o=16)
                dtb = dt_[:].unsqueeze(2).broadcast_to([128, 16, 64])
                T.op("act", lambda: nc.scalar.activation(out=dt_[:], in_=dt_[:], func=AF.Exp), R=[BP], W=[BP])
                T.op("dve", lambda: nc.vector.tensor_tensor(out=v3(ang), in0=v3(li), in1=dtb, op=ALU.mult), R=[BP], W=[BP])
                T.op("dve", lambda: nc.vector.tensor_tensor(out=v3(mag), in0=v3(lr), in1=dtb, op=ALU.mult), R=[BP], W=[BP])
                T.op("act", lambda: nc.scalar.activation(out=mag[:], in_=mag[:], func=AF.Exp), R=[BP], W=[BP])
                sincos(pp, ang[:], NP_, sn[:], cs[:], "p4sc", BP)
                T.B("p4sc", "tmp")
                T.barrier()
                TT = lambda o, a, b, op: T.op("dve", lambda: nc.vector.tensor_tensor(out=o[:], in0=a[:], in1=b[:], op=op), R=[BP], W=[BP])
                TT(ar, mag, cs, ALU.mult)
                TT(ai, mag, sn, ALU.mult)
                TT(den, lr, lr, ALU.mult)
                TT(t1_, li, li, ALU.mult)
                TT(den, den, t1_, ALU.add)
                T.op("dve", lambda: nc.vector.reciprocal(out=den[:], in_=den[:]), R=[BP], W=[BP])
                T.op("dve", lambda: nc.vector.tensor_scalar(out=ar[:], in0=ar[:], scalar1=-1.0, scalar2=None, op0=ALU.add), R=[BP], W=[BP])
                TT(fr, ar, lr, ALU.mult)
                TT(t1_, ai, li, ALU.mult)
                TT(fr, fr, t1_, ALU.add)
                TT(fr, fr, den, ALU.mult)
                TT(fi, ai, lr, ALU.mult)
                TT(t1_, ar, li, ALU.mult)
                TT(fi, fi, t1_, ALU.subtract)
                TT(fi, fi, den, ALU.mult)
                TT(t1_, fr, br, ALU.mult)
                TT(den, fi, bi, ALU.mult)
                TT(t1_, t1_, den, ALU.subtract)
                TT(ang, fr, bi, ALU.mult)
                TT(den, fi, br, ALU.mult)
                TT(ang, ang, den, ALU.add)
                for (dstt, c0_, src) in ((BB1, 0, t1_), (BB1, 64, ang), (BB2, 0, ang), (BB2, 64, t1_)):
                    T.op("dve", lambda: nc.vector.tensor_copy(out=dstt[:, :, c0_:c0_ + 64], in_=v3(src)), R=[BP], W=[BP])
            M1 = sb("p4M1", [128, G * 16], BF16, ph)
            M2 = sb("p4M2", [128, G * 16], BF16, ph)
            with contextlib.ExitStack() as pp:
                m1f = sb("p4m1f", [128, G * 16], F32, pp)
                m2f = sb("p4m2f", [128, G * 16], F32, pp)
                T.dma("sp", m1f[0:64, :], s_crT, W=[BP])
                T.dma("sp", m1f[64:128, :], s_ciT, W=[BP])
                T.dma("sp", m2f[0:64, :], s_ciT, W=[BP])
                T.dma("sp", m2f[64:128, :], s_crT, W=[BP])
                T.op("dve", lambda: nc.vector.tensor_copy(out=M1[0:64, :], in_=m1f[0:64, :]), R=[BP], W=[BP])
                T.op("dve", lambda: nc.vector.tensor_scalar(out=M1[64:128, :], in0=m1f[64:128, :], scalar1=-1.0, scalar2=None, op0=ALU.mult), R=[BP], W=[BP])
                T.op("dve", lambda: nc.vector.tensor_scalar(out=M2[0:64, :], in0=m2f[0:64, :], scalar1=-1.0, scalar2=None, op0=ALU.mult), R=[BP], W=[BP])
                T.op("dve", lambda: nc.vector.tensor_copy(out=M2[64:128, :], in_=m2f[64:128, :]), R=[BP], W=[BP])
            th = sb("p4th", [128, G], F32, ph)
            rho = sb("p4rho", [128, G], F32, ph)
            dts = sb("p4dts", [128, G], F32, ph)
            sgn = sb("p4sgn", [128, 1], F32, ph)
            rowmask = sb("p4rm", [128, 8], F32, ph)
            e16 = sb("p4e16", [128, 16], F32, ph)
            dsk = sb("p4dsk", [128, 16], F32, ph)
            t0r = sb("p4t0", [128, 64], F32, ph)
            t1r = sb("p4t1r", [128, 32], F32, ph)
            for (d_, s_) in ((th, s_liT), (rho, s_lrT), (dts, s_lsT), (sgn, c_sgn), (rowmask, c_rowmask), (e16, c_e16), (dsk, s_d), (t0r, c_t0), (t1r, c_t1)):
                T.dma("sp", d_[:], s_, W=[BP])
            T.op("act", lambda: nc.scalar.activation(out=dts[:], in_=dts[:], func=AF.Exp), R=[BP], W=[BP])
            T.op("dve", lambda: nc.vector.tensor_tensor(out=th[:], in0=th[:], in1=dts[:], op=ALU.mult), R=[BP], W=[BP])
            T.op("dve", lambda: nc.vector.tensor_scalar(out=th[:], in0=th[:], scalar1=sgn[:, 0:1], scalar2=None, op0=ALU.mult), R=[BP], W=[BP])
            T.op("dve", lambda: nc.vector.tensor_tensor(out=rho[:], in0=rho[:], in1=dts[:], op=ALU.mult), R=[BP], W=[BP])
            T.op("act", lambda: nc.scalar.activation(out=rho[:], in_=rho[:], func=AF.Exp), R=[BP], W=[BP])

            thi = sb("p4thi", [128, G], F32, ph)
            nrt = sb("p4nrt", [128, G], F32, ph)
            one_t = sb("p4one", [128, 1], F32, ph)
            ttab = sb("p4tt", [128, TALL], F32, ph)
            T.dma("sp", ttab[:], c_tt, W=[BP])
            T.op("dve", lambda: nc.vector.memset(one_t[:], 1.0), W=[BP])
            T.op("dve", lambda: nc.vector.tensor_scalar(out=thi[:], in0=th[:], scalar1=1.0 / TWO_PI, scalar2=None, op0=ALU.mult), R=[BP], W=[BP])
            T.op("dve", lambda: nc.vector.reciprocal(out=nrt[:], in_=th[:]), R=[BP], W=[BP])
            T.op("dve", lambda: nc.vector.tensor_scalar(out=nrt[:], in0=nrt[:], scalar1=-TWO_PI, scalar2=None, op0=ALU.mult), R=[BP], W=[BP])
            T.barrier()

            I32 = mybir.dt.int32
            PI_SAFE = 3.1415925
            uo = [sb(f"p4u{i}", [128, TALL], BF16, ph) for i in range(2)]
            ki = sb("p4ki", [128, TALL], I32, ph)
            uu = sb("p4uu", [128, TALL], F32, ph)
            sh = sb("p4sh", [128, TALL], F32, ph)
            Ec = [sb(f"p4Ec{i}", [128, TALL], F32, ph) for i in range(3)]
            Es = [sb(f"p4Es{i}", [128, TALL], F32, ph) for i in range(3)]
            tB = sb("p4tB", [128, TALL], F32, ph)
            Sp = [sb(f"p4Sp{i}", [128, TALL], F32, ph) for i in range(2)]
            Wt = [sb(f"p4W{i}", [128, TALL], F32, ph) for i in range(2)]
            Q1 = [sb(f"p4Q1{i}", [128, TOK], BF16, ph) for i in range(2)]
            Q2 = [sb(f"p4Q2{i}", [128, TOK], BF16, ph) for i in range(2)]
            Bp1 = [sb(f"p4Bp1{i}", [128, 128], BF16, ph) for i in range(2)]
            Bp2 = [sb(f"p4Bp2{i}", [128, 128], BF16, ph) for i in range(2)]
            Dp = [sb(f"p4Dp{i}", [128, 16], BF16, ph) for i in range(2)]
            yst = [sb(f"p4y{i}", [16, TOK], F32, ph) for i in range(2)]

            def ssm_tables(g):
                gb = g % 3
                BE, Bk, Buu, Bsh = T.B("p4E", gb), T.B("p4ki"), T.B("p4uu"), T.B("p4sh")
                T.op("dve", lambda: nc.vector.tensor_scalar(out=ki[:], in0=ttab[:], scalar1=thi[:, g:g + 1], scalar2=None, op0=ALU.mult), R=[], W=[Bk])
                T.op("dve", lambda: nc.vector.scalar_tensor_tensor(out=uu[:], in0=ttab[:], scalar=thi[:, g:g + 1], in1=ki[:], op0=ALU.mult, op1=ALU.subtract), R=[Bk], W=[Buu])
                T.op("act", lambda: nc.scalar.activation(out=Es[gb][:], in_=uu[:], func=AF.Sin, scale=TWO_PI * (1.0 - 1e-6)), R=[Buu], W=[BE])
                T.op("act", lambda: nc.scalar.activation(out=sh[:], in_=uu[:], func=AF.Sin, scale=math.pi * (1.0 - 1e-6)), R=[Buu], W=[Bsh])
                T.op("act", lambda: nc.scalar.activation(out=sh[:], in_=sh[:], func=AF.Square), R=[Bsh], W=[Bsh])
                T.op("act", lambda: nc.scalar.activation(out=Ec[gb][:], in_=sh[:], func=AF.Identity, scale=-2.0, bias=one_t[:, 0:1]), R=[Bsh], W=[BE])

            def ssm_bu(g, ub, Bu, hf):
                gb = g % 2
                BBp = T.B("p4Bp", gb)
                for ordr in range(2):
                    for q2 in range(2):
                        bk = ordr * 2 + q2
                        cc = hf * 1024 + q2 * 512
                        lh = Bp1[gb] if ordr == 0 else Bp2[gb]
                        T.op("pe", lambda: nc.tensor.matmul(bank(bk), lh[:], uo[ub][:, cc:cc + 512], start=True, stop=True), R=[BBp, Bu], W=[pb(bk)])

            def ssm_front_a(g, ub, Bu):
                o, gl, gb = g // 8, g % 8, g % 2
                BBp = T.B("p4Bp", gb)
                T.op("pool", lambda: nc.gpsimd.tensor_scalar(out=Bp1[gb][:], in0=BB1[:, o, :], scalar1=rowmask[:, gl:gl + 1], scalar2=1.0, op0=ALU.mult, op1=ALU.mult), R=[], W=[BBp])
                T.op("pool", lambda: nc.gpsimd.tensor_scalar(out=Bp2[gb][:], in0=BB2[:, o, :], scalar1=rowmask[:, gl:gl + 1], scalar2=1.0, op0=ALU.mult, op1=ALU.mult), R=[], W=[BBp])
                T.op("pool", lambda: nc.gpsimd.tensor_scalar(out=Dp[gb][:], in0=e16[:], scalar1=dsk[:, o:o + 1], scalar2=rowmask[:, gl:gl + 1], op0=ALU.mult, op1=ALU.mult), R=[], W=[BBp])
                ssm_bu(g, ub, Bu, 0)

            def ssm_front_b(g, ub, Bu):
                gb = g % 2
                ge = g % 3
                BE, BS = T.B("p4E", ge), T.B("p4Sp", gb)
                for hf in range(2):
                    if hf == 1:
                        ssm_bu(g, ub, Bu, 1)
                    hs = slice(hf * 1024, (hf + 1) * 1024)
                    T.op("dve", lambda: nc.vector.tensor_tensor(out=Sp[gb][:, hs], in0=PS[:, 0:1024], in1=Ec[ge][:, hs], op=ALU.mult), R=[pb(0), pb(1), BE], W=[BS])
                    T.op("dve", lambda: nc.vector.tensor_tensor(out=tB[:, hs], in0=PS[:, 1024:2048], in1=Es[ge][:, hs], op=ALU.mult), R=[pb(2), pb(3), BE], W=[T.B("p4tB")])
                    T.op("pool", lambda: nc.gpsimd.tensor_tensor(out=Sp[gb][:, hs], in0=Sp[gb][:, hs], in1=tB[:, hs], op=ALU.add), R=[T.B("p4tB")], W=[BS])

            def ssm_back(g, ub, Bu):
                gb = g % 2
                ge = g % 3
                BBp, BE, BS, BW, BQ = T.B("p4Bp", gb), T.B("p4E", ge), T.B("p4Sp", gb), T.B("p4W", gb), T.B("p4Q", gb)
                T.op("dve", lambda: nc.vector.tensor_tensor_scan(out=Wt[gb][:], data0=rho[:, g:g + 1].broadcast_to([128, TALL]), data1=Sp[gb][:], initial=0.0, op0=ALU.mult, op1=ALU.add),
                     R=[BS], W=[BW])
                T.op("pool", lambda: nc.gpsimd.tensor_tensor(out=Q1[gb][:], in0=Wt[gb][:, TOK:TALL], in1=Ec[ge][:, TOK:TALL], op=ALU.mult), R=[BW, BE], W=[BQ])
                T.op("pool", lambda: nc.gpsimd.tensor_tensor(out=Q2[gb][:], in0=Wt[gb][:, TOK:TALL], in1=Es[ge][:, TOK:TALL], op=ALU.mult), R=[BW, BE], W=[BQ])
                for q2 in range(2):
                    bk = 4 + q2
                    cs_ = slice(q2 * 512, (q2 + 1) * 512)
                    T.op("pe", lambda: nc.tensor.matmul(bank(bk)[0:16, :], M1[:, g * 16:(g + 1) * 16], Q1[gb][:, cs_], start=True, stop=False), R=[BQ], W=[pb(bk)])
                    T.op("pe", lambda: nc.tensor.matmul(bank(bk)[0:16, :], M2[:, g * 16:(g + 1) * 16], Q2[gb][:, cs_], start=False, stop=False), R=[BQ], W=[pb(bk)])
                    T.op("pe", lambda: nc.tensor.matmul(bank(bk)[0:16, :], Dp[gb][:], uo[ub][:, TOK + q2 * 512:TOK + (q2 + 1) * 512], start=False, stop=True), R=[BBp, Bu], W=[pb(bk)])
                By = T.B("p4y", gb)
                T.op("act", lambda: nc.scalar.activation(out=yst[gb][:], in_=PS[0:16, 2048:3072], func=AF.Copy), R=[pb(4), pb(5)], W=[By])
                T.dma("sp", ypreT[g * 16:(g + 1) * 16, :], yst[gb][:], R=[By], W=[T.B("ypreT", g)])

            ssm_tables(0)
            ssm_tables(1)

            def ssm_u(g):
                ub = (g // 8) % 2
                Bu = T.B("p4u", ub)
                if g % 8 == 0:
                    T.dma("sp", uo[ub][:], uT[(g // 8) * 128:(g // 8 + 1) * 128, :], W=[Bu])
                return ub, Bu

            ub0, Bu0 = ssm_u(0)
            ssm_front_a(0, ub0, Bu0)
            cur = (ub0, Bu0)
            for g in range(G):
                ub, Bu = cur
                ssm_front_b(g, ub, Bu)
                if g + 2 < G:
                    ssm_tables(g + 2)
                if g + 1 < G:
                    nxt = ssm_u(g + 1)
                    ssm_front_a(g + 1, nxt[0], nxt[1])
                ssm_back(g, ub, Bu)
                if g + 1 < G:
                    cur = nxt
        T.barrier()
        for ph in _phase(4):
            yt_ = [sb(f"p4gy{i}", [128, TOK], F32, ph) for i in range(2)]
            g1 = sb("p4g1", [128, TOK], F32, ph)
            g2 = sb("p4g2", [128, TOK], F32, ph)
            go = [sb(f"p4go{i}", [128, TOK], BF16, ph) for i in range(2)]
            for o in range(16):
                b_ = o % 2
                By, Bg, Bo = T.B("p4gy", b_), T.B("p4g"), T.B("p4go", b_)
                T.dma("sp", yt_[b_][:], ypreT[o * 128:(o + 1) * 128, :], W=[By])
                T.op("dve", lambda: nc.vector.tensor_tensor(out=g1[:], in0=yt_[b_][:], in1=yt_[b_][:], op=ALU.mult), R=[By], W=[Bg])
                T.op("dve", lambda: nc.vector.tensor_scalar(out=g1[:], in0=g1[:], scalar1=0.044715, scalar2=1.0, op0=ALU.mult, op1=ALU.add), R=[Bg], W=[Bg])
                T.op("dve", lambda: nc.vector.tensor_tensor(out=g1[:], in0=g1[:], in1=yt_[b_][:], op=ALU.mult), R=[Bg, By], W=[Bg])
                T.op("act", lambda: nc.scalar.activation(out=g2[:], in_=g1[:], func=AF.Sigmoid, scale=2.0 * math.sqrt(2.0 / math.pi)), R=[Bg], W=[T.B("p4g2")])
                T.op("dve", lambda: nc.vector.tensor_tensor(out=go[b_][:], in0=g2[:], in1=yt_[b_][:], op=ALU.mult), R=[T.B("p4g2"), By], W=[Bo])
                T.dma("sp", ygT[o * 128:(o + 1) * 128, :], go[b_][:], R=[Bo], W=[T.B("ygT", o)])
        T.barrier()
        if STOP_AFTER <= 4:
            T.final_wait()
            return nc

        for ph in _phase(5):
            act = sb("p5act", [128, 16, TOK], BF16, ph)
            actb = load_act(act, ygT, 2048, 0, TOK, "p5act")
            wa = [sb(f"p5wa{i}", [128, 16, 512], BF16, ph) for i in range(2)]
            wb_ = [sb(f"p5wb{i}", [128, 16, 512], BF16, ph) for i in range(2)]
            sg = sb("p5sg", [128, 512], F32, ph)
            st = [sb(f"p5st{i}", [128, TOK], BF16, ph) for i in range(2)]
            n_ = 0
            for cg in range(4):
                wal = load_w(wa[cg % 2], w_glu, 2048, cg * 512, 512, ("p5wa", cg % 2))
                wbl = load_w(wb_[cg % 2], w_glu, 2048, 2048 + cg * 512, 512, ("p5wb", cg % 2))
                for nch in range(4):
                    stb = T.B("p5st", n_ % 2)
                    for tt in range(2):
                        bkA, bkB = (n_ * 4 + tt * 2) % 8, (n_ * 4 + tt * 2 + 1) % 8
                        for kc in range(16):
                            T.op("pe", lambda: nc.tensor.matmul(bank(bkA), wa[cg % 2][:, kc, nch * 128:(nch + 1) * 128], act[:, kc, tt * 512:(tt + 1) * 512], start=(kc == 0), stop=(kc == 15)), R=actb + wal, W=[pb(bkA)])
                        for kc in range(16):
                            T.op("pe", lambda: nc.tensor.matmul(bank(bkB), wb_[cg % 2][:, kc, nch * 128:(nch + 1) * 128], act[:, kc, tt * 512:(tt + 1) * 512], start=(kc == 0), stop=(kc == 15)), R=actb + wbl, W=[pb(bkB)])
                        T.op("act", lambda: nc.scalar.activation(out=sg[:], in_=bank(bkB), func=AF.Sigmoid), R=[pb(bkB)], W=[T.B("p5sg")])
                        T.op("dve", lambda: nc.vector.tensor_tensor(out=st[n_ % 2][:, tt * 512:(tt + 1) * 512], in0=bank(bkA), in1=sg[:], op=ALU.mult), R=[pb(bkA), T.B("p5sg")], W=[stb])
                    r0 = cg * 512 + nch * 128
                    T.dma("sp", yssmT[r0:r0 + 128, :], st[n_ % 2][:], R=[stb], W=[T.B("yssmT", r0)])
                    n_ += 1
        T.barrier()
        for ph in _phase(5):
            actA = sb("p5aA", [128, 16, TOK], BF16, ph)
            actS = sb("p5aS", [128, 16, TOK], BF16, ph)
            aAb = load_act(actA, yattnT, 2048, 0, TOK, "p5aA")
            aSb = load_act(actS, yssmT, 2048, 0, TOK, "p5aS")
            wa = [sb(f"p5mwa{i}", [128, 16, 512], BF16, ph) for i in range(2)]
            wb_ = [sb(f"p5mwb{i}", [128, 16, 512], BF16, ph) for i in range(2)]
            ga_t = [sb(f"p5ga{i}", [128, TOK], BF16, ph) for i in range(2)]
            gs_t = [sb(f"p5gs{i}", [128, TOK], BF16, ph) for i in range(2)]
            t1_ = sb("p5mt1", [128, 512], F32, ph)
            t2_ = sb("p5mt2", [128, 512], F32, ph)
            st = [sb(f"p5mst{i}", [128, TOK], BF16, ph) for i in range(2)]
            n_ = 0
            for cg in range(8):
                wal = load_w(wa[cg % 2], w_oa, 2048, cg * 512, 512, ("p5wa", cg % 2))
                wbl = load_w(wb_[cg % 2], w_os, 2048, cg * 512, 512, ("p5wb", cg % 2))
                for nch in range(4):
                    r0 = cg * 512 + nch * 128
                    stb = T.B("p5st", n_ % 2)
                    Bga, Bgs = T.B("p5ga", n_ % 2), T.B("p5gs", n_ % 2)
                    T.dma("sp", ga_t[n_ % 2][:], sga[r0:r0 + 128, :], W=[Bga])
                    T.dma("sp", gs_t[n_ % 2][:], sgs[r0:r0 + 128, :], W=[Bgs])
                    for tt in range(2):
                        bkA, bkB = (n_ * 4 + tt * 2) % 8, (n_ * 4 + tt * 2 + 1) % 8
                        for kc in range(16):
                            T.op("pe", lambda: nc.tensor.matmul(bank(bkA), wa[cg % 2][:, kc, nch * 128:(nch + 1) * 128], actA[:, kc, tt * 512:(tt + 1) * 512], start=(kc == 0), stop=(kc == 15)), R=aAb + wal, W=[pb(bkA)])
                        for kc in range(16):
                            T.op("pe", lambda: nc.tensor.matmul(bank(bkB), wb_[cg % 2][:, kc, nch * 128:(nch + 1) * 128], actS[:, kc, tt * 512:(tt + 1) * 512], start=(kc == 0), stop=(kc == 15)), R=aSb + wbl, W=[pb(bkB)])
                        ts_ = slice(tt * 512, (tt + 1) * 512)
                        T.op("dve", lambda: nc.vector.tensor_tensor(out=t1_[:], in0=bank(bkA), in1=ga_t[n_ % 2][:, ts_], op=ALU.mult), R=[pb(bkA), Bga], W=[T.B("p5mt1")])
                        T.op("dve", lambda: nc.vector.tensor_tensor(out=t2_[:], in0=bank(bkB), in1=gs_t[n_ % 2][:, ts_], op=ALU.mult), R=[pb(bkB), Bgs], W=[T.B("p5mt2")])
                        T.op("pool", lambda: nc.gpsimd.tensor_tensor(out=st[n_ % 2][:, ts_], in0=t1_[:], in1=t2_[:], op=ALU.add), R=[T.B("p5mt1"), T.B("p5mt2")], W=[stb])
                    T.dma("sp", mixedT[r0:r0 + 128, :], st[n_ % 2][:], R=[stb], W=[T.B("mixedT", r0)])
                    n_ += 1
        T.barrier()
        for ph in _phase(5):
            act = sb("p5oact", [128, 32, TOK], BF16, ph)
            actb = load_act(act, mixedT, D, 0, TOK, "p5act")
            wbuf = [sb(f"p5w{i}", [128, 32, 512], BF16, ph) for i in range(2)]
            xr = [sb(f"p5x{i}", [128, 512], F32, ph) for i in range(2)]
            ho = [sb(f"p5h{i}", [128, 512], F32, ph) for i in range(2)]
            n_ = 0
            for cg in range(8):
                wbl = load_w(wbuf[cg % 2], w_out, D, cg * 512, 512, ("p5w", cg % 2))
                for tch in range(8):
                    bk = n_ % 8
                    Bx, Bh = T.B("p5x", n_ % 2), T.B("p5h", n_ % 2)
                    T.dma("sp", xr[n_ % 2][:], xin[TOK + tch * 128:TOK + (tch + 1) * 128, cg * 512:(cg + 1) * 512], W=[Bx])
                    for kc in range(32):
                        T.op("pe", lambda: nc.tensor.matmul(bank(bk), act[:, kc, tch * 128:(tch + 1) * 128], wbuf[cg % 2][:, kc, :], start=(kc == 0), stop=(kc == 31)), R=actb + wbl, W=[pb(bk)])
                    T.op("dve", lambda: nc.vector.tensor_tensor(out=ho[n_ % 2][:], in0=bank(bk), in1=xr[n_ % 2][:], op=ALU.add), R=[pb(bk), Bx], W=[Bh])
                    T.dma("sp", h1[tch * 128:(tch + 1) * 128, cg * 512:(cg + 1) * 512], ho[n_ % 2][:], R=[Bh], W=[T.B("h1", tch, cg)])
                    n_ += 1
        T.barrier()
        if STOP_AFTER <= 5:
            T.final_wait()
            return nc

        for ph in _phase(6):
            selw = sb("p6selw", [128, 8, NSLOT], BF16, ph)
            with contextlib.ExitStack() as p6a:
                gf = sb("p6gf", [128, D], F32, p6a)
                T.dma("sp", gf[:], gffn, W=[T.B("gf")])
                wr = sb("p6wr", [128, 32, 72], BF16, p6a)
                T.dma("pool", wr[:], w_rt.rearrange("(c p) n -> p c n", p=128), W=[T.B("wr")], max_dma_last_dim=4096)
                brt = sb("p6brt", [128, 72], F32, p6a)
                T.dma("sp", brt[:], b_rt, W=[T.B("brt")])
                iot = sb("p6iota", [128, NSLOT], F32, p6a)
                T.dma("sp", iot[:], c_iota, W=[T.B("iota")])
                ecap = sb("p6ecap", [128, NE], F32, p6a)
                T.dma("sp", ecap[:], c_ecap, W=[T.B("ecap")])
                lt = sb("p6lt", [128, 128], BF16, p6a)
                T.dma("sp", lt[:], c_lt, W=[T.B("lt")])
                ones = sb("p6ones", [128, 128], BF16, p6a)
                T.dma("sp", ones[:], c_ones, W=[T.B("ones")])
                ht = [sb(f"p6h{i}", [128, D], F32, p6a) for i in range(2)]
                hnb = [sb(f"p6hn{i}", [128, D], BF16, p6a) for i in range(2)]
                hnT = sb("p6hnT", [128, 32, 128], BF16, p6a)
                ss = sb("p6ss", [128, 1], F32, p6a)
                rs = sb("p6rs", [128, 1], F32, p6a)
                lg = sb("p6lg", [128, 72], F32, p6a)
                m8 = sb("p6m8", [128, 8], F32, p6a)
                eg = sb("p6eg", [128, 8], F32, p6a)
                sumg = sb("p6sumg", [128, 1], F32, p6a)
                ohp = sb("p6ohp", [128, 8], F32, p6a)
                lem = sb("p6lem", [128, 64], F32, p6a)
                t8 = sb("p6t8", [128, 8], F32, p6a)
                dv = sb("p6dv", [128, 1], F32, p6a)
                A1 = sb("p6A1", [128, 8, 64], F32, p6a)
                A2 = sb("p6A2", [128, 8, 64], F32, p6a)
                Ab = sb("p6Ab", [128, 8, 64], BF16, p6a)
                w1 = sb("p6w1", [128, 8], F32, p6a)
                w2 = sb("p6w2", [128, 8], F32, p6a)
                for t in range(8):
                    b_ = t % 2
                    Bh, Bn = T.B("p6h", b_), T.B("p6hn", b_)
                    T.dma("sp", ht[b_][:], h1[t * 128:(t + 1) * 128, :], W=[Bh])
                    rms_rstd(p6a, ht[b_][:], Bh, hnb[b_][:], Bn, ss[:], rs[:], "p6")
                    T.op("dve", lambda: nc.vector.scalar_tensor_tensor(out=hnb[b_][:], in0=ht[b_][:], scalar=rs[:, 0:1], in1=gf[:], op0=ALU.mult, op1=ALU.mult),
                         R=[Bh, T.B("p6", "rs"), T.B("gf")], W=[Bn])
                    T.dma("sp", hn[t * 128:(t + 1) * 128, :], hnb[b_][:], R=[Bn], W=[T.B("hn", t)])
                    for q4 in range(4):
                        bk = q4
                        pview = bank(bk).bitcast(BF16)
                        for c8 in range(8):
                            c = q4 * 8 + c8
                            T.op("pe", lambda: nc.tensor.transpose(pview[:, c8 * 128:(c8 + 1) * 128], hnb[b_][:, c * 128:(c + 1) * 128], ident[:]), R=[Bn, T.B("ident")], W=[pb(bk)])
                        copy_op(evac_engine(), hnT[:, q4 * 8:(q4 + 1) * 8, :], pview.rearrange("p (c t) -> p c t", c=8), [pb(bk)], [T.B("hnT", q4)])
                    for kc in range(32):
                        T.op("pe", lambda: nc.tensor.matmul(bank(4)[:, 0:72], hnT[:, kc, :], wr[:, kc, :], start=(kc == 0), stop=(kc == 31)),
                             R=[T.B("hnT", kc // 8), T.B("wr")], W=[pb(4)])
                    BR = T.B("p6route")
                    T.op("dve", lambda: nc.vector.tensor_tensor(out=lg[:], in0=bank(4)[:, 0:72], in1=brt[:], op=ALU.add), R=[pb(4), T.B("brt")], W=[BR])
                    T.op("dve", lambda: nc.vector.max(out=m8[:], in_=lg[:, 0:8]), R=[BR], W=[BR])
                    T.op("dve", lambda: nc.vector.tensor_scalar(out=eg[:], in0=lg[:, 0:8], scalar1=m8[:, 0:1], scalar2=None, op0=ALU.subtract), R=[BR], W=[BR])
                    T.op("act", lambda: nc.scalar.activation(out=eg[:], in_=eg[:], func=AF.Exp, accum_out=sumg[:]), R=[BR], W=[BR])
                    T.op("dve", lambda: nc.vector.reciprocal(out=sumg[:], in_=sumg[:]), R=[BR], W=[BR])
                    T.op("dve", lambda: nc.vector.tensor_scalar(out=ohp[:], in0=lg[:, 0:8], scalar1=m8[:, 0:1], scalar2=None, op0=ALU.is_equal), R=[BR], W=[BR])
                    T.op("dve", lambda: nc.vector.tensor_scalar(out=ohp[:], in0=ohp[:], scalar1=1e30, scalar2=-1e30, op0=ALU.mult, op1=ALU.add), R=[BR], W=[BR])
                    T.op("dve", lambda: nc.vector.tensor_tensor(out=lem[:].rearrange("p (g e) -> p g e", g=8), in0=lg[:, 8:72].rearrange("p (g e) -> p g e", g=8),
                                                                 in1=ohp[:].unsqueeze(2).broadcast_to([128, 8, 8]), op=ALU.add), R=[BR], W=[BR])
                    T.op("dve", lambda: nc.vector.max(out=t8[:], in_=lem[:]), R=[BR], W=[BR])
                    T.op("dve", lambda: nc.vector.tensor_tensor(out=dv[:], in0=t8[:, 0:1], in1=t8[:, 1:2], op=ALU.subtract), R=[BR], W=[BR])
                    T.op("act", lambda: nc.scalar.activation(out=dv[:], in_=dv[:], func=AF.Sigmoid), R=[BR], W=[BR])
                    T.op("dve", lambda: nc.vector.tensor_tensor(out=w1[:, t:t + 1], in0=dv[:], in1=sumg[:], op=ALU.mult), R=[BR], W=[T.B("p6w")])
                    T.op("dve", lambda: nc.vector.tensor_tensor(out=w2[:, t:t + 1], in0=sumg[:], in1=w1[:, t:t + 1], op=ALU.subtract), R=[BR, T.B("p6w")], W=[T.B("p6w")])
                    T.op("dve", lambda: nc.vector.tensor_scalar(out=A1[:, t, :], in0=lem[:], scalar1=t8[:, 0:1], scalar2=None, op0=ALU.is_equal), R=[BR], W=[T.B("p6A")])
                    T.op("dve", lambda: nc.vector.tensor_scalar(out=A2[:, t, :], in0=lem[:], scalar1=t8[:, 1:2], scalar2=None, op0=ALU.is_equal), R=[BR], W=[T.B("p6A")])
                    T.op("dve", lambda: nc.vector.tensor_tensor(out=Ab[:, t, :], in0=A1[:, t, :], in1=A2[:, t, :], op=ALU.add), R=[T.B("p6A")], W=[T.B("p6Ab")])
                cum = sb("p6cum", [128, 64], F32, p6a)
                tq = sb("p6tq", [128, 64], F32, p6a)
                d1 = sb("p6d1", [128, 1], F32, p6a)
                ok = sb("p6ok", [128, 1], F32, p6a)
                dd = sb("p6dd", [128, 4], F32, p6a)
                tmpS = sb("p6tmpS", [128, NSLOT], BF16, p6a)
                for t in range(8):
                    for tp in range(t + 1):
                        lh = ones if tp < t else lt
                        T.op("pe", lambda: nc.tensor.matmul(bank(5)[:, 0:64], lh[:], Ab[:, tp, :], start=(tp == 0), stop=(tp == t)),
                             R=[T.B("p6Ab"), T.B("ones"), T.B("lt")], W=[pb(5)])
                    BC = T.B("p6cum")
                    T.op("dve", lambda: nc.vector.tensor_copy(out=cum[:], in_=bank(5)[:, 0:64]), R=[pb(5)], W=[BC])
                    for k_, Ak in enumerate((A1, A2)):
                        T.op("dve", lambda: nc.vector.tensor_tensor(out=tq[:], in0=cum[:], in1=ecap[:], op=ALU.add), R=[BC, T.B("ecap")], W=[BC])
                        T.op("dve", lambda: nc.vector.tensor_tensor(out=tq[:], in0=tq[:], in1=Ak[:, t, :], op=ALU.mult), R=[BC, T.B("p6A")], W=[BC])
                        T.op("dve", lambda: nc.vector.tensor_reduce(out=d1[:], in_=tq[:], axis=AX.X, op=ALU.add), R=[BC], W=[BC])
                        T.op("dve", lambda: nc.vector.tensor_scalar(out=tq[:], in0=cum[:], scalar1=float(CAP), scalar2=None, op0=ALU.is_lt), R=[BC], W=[BC])
                        T.op("dve", lambda: nc.vector.tensor_tensor(out=tq[:], in0=tq[:], in1=Ak[:, t, :], op=ALU.mult), R=[BC, T.B("p6A")], W=[BC])
                        T.op("dve", lambda: nc.vector.tensor_reduce(out=ok[:], in_=tq[:], axis=AX.X, op=ALU.add), R=[BC], W=[BC])
                        T.op("dve", lambda: nc.vector.tensor_scalar(out=d1[:], in0=d1[:], scalar1=1.0, scalar2=ok[:, 0:1], op0=ALU.add, op1=ALU.mult), R=[BC], W=[BC])
                        T.op("dve", lambda: nc.vector.tensor_scalar(out=dd[:, k_:k_ + 1], in0=d1[:], scalar1=-1.0, scalar2=None, op0=ALU.add), R=[BC], W=[BC])
                    wk1, wk2 = w1[:, t:t + 1], w2[:, t:t + 1]
                    T.op("dve", lambda: nc.vector.tensor_scalar(out=tmpS[:], in0=iot[:], scalar1=dd[:, 0:1], scalar2=wk1, op0=ALU.is_equal, op1=ALU.mult),
                         R=[BC, T.B("iota"), T.B("p6w")], W=[T.B("tmpS")])
                    T.op("dve", lambda: nc.vector.tensor_scalar(out=selw[:, t, :], in0=iot[:], scalar1=dd[:, 1:2], scalar2=wk2, op0=ALU.is_equal, op1=ALU.mult),
                         R=[BC, T.B("iota"), T.B("p6w")], W=[T.B("selw", t)])
                    T.op("pool", lambda: nc.gpsimd.tensor_tensor(out=selw[:, t, :], in0=selw[:, t, :], in1=tmpS[:], op=ALU.add), R=[T.B("tmpS")], W=[T.B("selw", t)])
            T.barrier()
            with contextlib.ExitStack() as p6c:
                selb = sb("p6selb", [128, 8, NSLOT], BF16, p6c)
                for t in range(8):
                    T.op("dve", lambda: nc.vector.tensor_scalar(out=selb[:, t, :], in0=selw[:, t, :], scalar1=0.0, scalar2=None, op0=ALU.is_gt), R=[T.B("selw", t)], W=[T.B("selb", t)])
                hcg = [sb(f"p6hcg{i}", [128, 8, 512], BF16, p6c) for i in range(2)]
                xst = [sb(f"p6xst{i}", [128, 512], BF16, p6c) for i in range(2)]
                n_ = 0
                for cg in range(8):
                    Bh = T.B("p6hcg", cg % 2)
                    T.dma("sp", hcg[cg % 2][:], hn.rearrange("(t p) f -> p t f", p=128)[:, :, cg * 512:(cg + 1) * 512], W=[Bh])
                    for s in range(32):
                        bk = n_ % 8
                        for t in range(8):
                            T.op("pe", lambda: nc.tensor.matmul(bank(bk), selb[:, t, s * 128:(s + 1) * 128], hcg[cg % 2][:, t, :], start=(t == 0), stop=(t == 7)),
                                 R=[T.B("selb", t), Bh], W=[pb(bk)])
                        Bx = T.B("p6xst", n_ % 2)
                        copy_op(evac_engine(), xst[n_ % 2][:], bank(bk), [pb(bk)], [Bx])
                        T.dma("sp", xs[s * 128:(s + 1) * 128, cg * 512:(cg + 1) * 512], xst[n_ % 2][:], R=[Bx], W=[T.B("xs", s, cg)])
                        n_ += 1
            T.barrier()
            with contextlib.ExitStack() as p6d:
                wg = sb("p6wg", [128, 32, 512], BF16, p6d)
                wu = sb("p6wu", [128, 32, 512], BF16, p6d)
                wd = sb("p6wd", [128, 4, D], BF16, p6d)
                xrow = [sb(f"p6xr{i}", [128, D], BF16, p6d) for i in range(2)]
                xT = [sb(f"p6xT{i}", [128, 32, 128], BF16, p6d) for i in range(2)]
                sgt = sb("p6sg", [64, 512], F32, p6d)
                hd = sb("p6hd", [64, 512], BF16, p6d)
                hT = sb("p6hT", [128, 4, 64], BF16, p6d)
                yst = [sb(f"p6ys{i}", [64, 2048], BF16, p6d) for i in range(2)]
                ny = 0
                for s in range(32):
                    sb_ = s % 2
                    Bxr = T.B("p6xr", sb_)
                    T.dma("sp", xrow[sb_][:], xs[s * 128:(s + 1) * 128, :], W=[Bxr])
                    for q4 in range(4):
                        bk = 4 + q4 % 2
                        pview = bank(bk).bitcast(BF16)
                        for c8 in range(8):
                            c = q4 * 8 + c8
                            T.op("pe", lambda: nc.tensor.transpose(pview[:, c8 * 128:(c8 + 1) * 128], xrow[sb_][:, c * 128:(c + 1) * 128], ident[:]), R=[Bxr, T.B("ident")], W=[pb(bk)])
                        copy_op(evac_engine(), xT[sb_][:, q4 * 8:(q4 + 1) * 8, :], pview.rearrange("p (c t) -> p c t", c=8), [pb(bk)], [T.B("p6xT", sb_, q4)])
                    BxT = [T.B("p6xT", sb_, q4) for q4 in range(4)]
                    for hf in range(2):
                        e = 2 * s + hf
                        wgl = load_w(wg, w_gate[e], D, 0, DE, ("p6wg",))
                        wul = load_w(wu, w_up[e], D, 0, DE, ("p6wu",))
                        wdl = load_w(wd, w_down[e], DE, 0, D, ("p6wd",))
                        for kc in range(32):
                            T.op("pe", lambda: nc.tensor.matmul(bank(0)[0:64, :], xT[sb_][:, kc, hf * 64:(hf + 1) * 64], wg[:, kc, :], start=(kc == 0), stop=(kc == 31)), R=BxT + [wgl[kc // 8]], W=[pb(0)])
                        for kc in range(32):
                            T.op("pe", lambda: nc.tensor.matmul(bank(1)[0:64, :], xT[sb_][:, kc, hf * 64:(hf + 1) * 64], wu[:, kc, :], start=(kc == 0), stop=(kc == 31)), R=BxT + [wul[kc // 8]], W=[pb(1)])
                        T.op("act", lambda: nc.scalar.activation(out=sgt[:], in_=bank(0)[0:64, :], func=AF.Silu), R=[pb(0)], W=[T.B("p6sg")])
                        T.op("dve", lambda: nc.vector.tensor_tensor(out=hd[:], in0=bank(1)[0:64, :], in1=sgt[:], op=ALU.mult), R=[pb(1), T.B("p6sg")], W=[T.B("p6hd")])
                        pv = bank(6).bitcast(BF16)
                        for kc in range(4):
                            T.op("pe", lambda: nc.tensor.transpose(pv[:, kc * 64:(kc + 1) * 64], hd[:, kc * 128:(kc + 1) * 128], ident[0:64, 0:64]), R=[T.B("p6hd"), T.B("ident")], W=[pb(6)])
                        copy_op("act", hT[:], pv[:, 0:256].rearrange("p (c t) -> p c t", c=4), [pb(6)], [T.B("p6hT")])
                        for half in range(2):
                            By = T.B("p6ys", ny % 2)
                            for c4 in range(4):
                                cgi = half * 4 + c4
                                bk = 2 + (cgi % 2)
                                for kc in range(4):
                                    T.op("pe", lambda: nc.tensor.matmul(bank(bk)[0:64, :], hT[:, kc, :], wd[:, kc, cgi * 512:(cgi + 1) * 512], start=(kc == 0), stop=(kc == 3)), R=[T.B("p6hT"), wdl[kc]], W=[pb(bk)])
                                copy_op(evac_engine(), yst[ny % 2][:, c4 * 512:(c4 + 1) * 512], bank(bk)[0:64, :], [pb(bk)], [By])
                            T.dma("sp", ys[e * 64:(e + 1) * 64, half * 2048:(half + 1) * 2048], yst[ny % 2][:], R=[By], W=[T.B("ys", e, half)])
                            ny += 1
            T.barrier()
            with contextlib.ExitStack() as p6e:
                swT = sb("p6swT", [128, 32, 8, 128], BF16, p6e)
                for s in range(32):
                    bk = s % 2
                    pv = bank(bk).bitcast(BF16)
                    for t in range(8):
                        T.op("pe", lambda: nc.tensor.transpose(pv[:, t * 128:(t + 1) * 128], selw[:, t, s * 128:(s + 1) * 128], ident[:]), R=[T.B("selw", t), T.B("ident")], W=[pb(bk)])
                    copy_op(evac_engine(), swT[:, s, :, :], pv.rearrange("p (t k) -> p t k", t=8), [pb(bk)], [T.B("swT", s)])
                ycg = [sb(f"p6ycg{i}", [128, 32, 512], BF16, p6e) for i in range(2)]
                hr = [sb(f"p6hr{i}", [128, 512], F32, p6e) for i in range(2)]
                ho = [sb(f"p6ho{i}", [128, 512], F32, p6e) for i in range(2)]
                n_ = 0
                swl = [T.B("swT", s) for s in range(32)]
                for cg in range(8):
                    Byc = T.B("p6ycg", cg % 2)
                    T.dma("sp", ycg[cg % 2][:], ys.rearrange("(s p) f -> p s f", p=128)[:, :, cg * 512:(cg + 1) * 512], W=[Byc])
                    for t in range(8):
                        bk = 2 + n_ % 6
                        Bhr, Bho = T.B("p6hr", n_ % 2), T.B("p6ho", n_ % 2)
                        T.dma("sp", hr[n_ % 2][:], h1[t * 128:(t + 1) * 128, cg * 512:(cg + 1) * 512], W=[Bhr])
                        for s in range(32):
                            T.op("pe", lambda: nc.tensor.matmul(bank(bk), swT[:, s, t, :], ycg[cg % 2][:, s, :], start=(s == 0), stop=(s == 31)), R=swl + [Byc], W=[pb(bk)])
                        T.op("dve", lambda: nc.vector.tensor_tensor(out=ho[n_ % 2][:], in0=bank(bk), in1=hr[n_ % 2][:], op=ALU.add), R=[pb(bk), Bhr], W=[Bho])
                        T.dma("sp", h2[t * 128:(t + 1) * 128, cg * 512:(cg + 1) * 512], ho[n_ % 2][:], R=[Bho], W=[T.B("h2", t, cg)])
                        n_ += 1
        T.barrier()
        for ph in _phase(7):
            gfi = sb("p7g", [128, D], F32, ph)
            T.dma("sp", gfi[:], gfin, W=[T.B("gfi")])
            ht = [sb(f"p7h{i}", [128, D], F32, ph) for i in range(2)]
            ot = [sb(f"p7o{i}", [128, D], F32, ph) for i in range(2)]
            junk = sb("p7junk", [128, D], BF16, ph)
            ss = sb("p7ss", [128, 1], F32, ph)
            rs = sb("p7rs", [128, 1], F32, ph)
            for t in range(8):
                b_ = t % 2
                Bh, Bo = T.B("p7h", b_), T.B("p7o", b_)
                T.dma("sp", ht[b_][:], h2[t * 128:(t + 1) * 128, :], W=[Bh])
                rms_rstd(ph, ht[b_][:], Bh, junk[:], T.B("p7junk"), ss[:], rs[:], "p7")
                T.op("dve", lambda: nc.vector.scalar_tensor_tensor(out=ot[b_][:], in0=ht[b_][:], scalar=rs[:, 0:1], in1=gfi[:], op0=ALU.mult, op1=ALU.mult),
                     R=[Bh, T.B("p7", "rs"), T.B("gfi")], W=[Bo])
                T.dma("sp", out[t * 128:(t + 1) * 128, :], ot[b_][:], R=[Bo], W=[T.B("out", t)])
        T.final_wait()
    return nc


def _consts(half):
    bf = ml_dtypes.bfloat16
    c = {}
    c["c_ident"] = np.eye(128, dtype=np.float32).astype(bf)
    p = np.arange(128)[:, None]
    i = np.arange(2048)[None, :]
    tbv = (p + 1920 - i).astype(np.float32)
    tbv[tbv < 0] = 1.0e6
    c["c_tb"] = tbv
    c["c_lt"] = (np.arange(128)[:, None] < np.arange(128)[None, :]).astype(np.float32).astype(bf)
    c["c_ones"] = np.ones((128, 128), np.float32).astype(bf)
    c["c_iota"] = np.broadcast_to(np.arange(NSLOT, dtype=np.float32)[None, :], (128, NSLOT)).copy()
    c["c_ecap"] = np.broadcast_to((np.arange(NE, dtype=np.float32) * CAP)[None, :], (128, NE)).copy()
    rm = np.zeros((128, 8), np.float32)
    for gl in range(8):
        rm[gl * 16:(gl + 1) * 16, gl] = 1.0
    c["c_rowmask"] = rm
    c["c_e16"] = np.tile(np.eye(16, dtype=np.float32), (8, 1))
    sg = np.ones((128, 1), np.float32)
    sg[64:] = -1.0
    c["c_sgn"] = sg
    c["c_t0"] = np.broadcast_to(np.arange(64, dtype=np.float32)[None, :], (128, 64)).copy()
    c["c_t1"] = np.broadcast_to((64.0 * np.arange(32, dtype=np.float32))[None, :], (128, 32)).copy()
    c["c_tt"] = np.broadcast_to(np.arange(TALL, dtype=np.float32)[None, :], (128, TALL)).copy()
    penG = np.zeros((8, 8), np.float32)
    own = np.zeros((8, 8), np.float32)
    dis = np.zeros((8, 8), np.float32)
    for qt in range(8):
        j = qt // 2
        for n in range(8):
            if n < 4:
                allowed = (half == 1)
            else:
                allowed = (n - 4) < j
            if n == 4 + j:
                own[qt, n] = 1.0
                penG[qt, n] = -1e30
            elif not allowed:
                penG[qt, n] = -1e30
                dis[qt, n] = -30000.0
    c["c_penG"] = np.broadcast_to(penG.reshape(1, 64), (128, 64)).copy()
    c["c_own"] = np.broadcast_to(own.reshape(1, 64), (128, 64)).copy()
    c["c_dis"] = np.broadcast_to(dis.reshape(1, 64), (128, 64)).copy()
    return c


def _shared_inputs(inp):
    f = lambda a: np.ascontiguousarray(np.asarray(a, dtype=np.float32))
    s = {}
    s["gmixT"] = f(np.asarray(inp["g_mix"])[0].reshape(32, 128).T)
    s["w_in"] = f(inp["w_in"][0])
    s["w_glu"] = f(inp["w_glu"][0])
    s["w_o_attn"] = f(inp["w_o_attn"][0])
    s["w_o_ssm"] = f(inp["w_o_ssm"][0])
    s["w_out"] = f(inp["w_out"][0])
    s["gffn"] = f(np.broadcast_to(np.asarray(inp["g_ffn"])[0][None, :], (128, D)))
    s["gfin"] = f(np.broadcast_to(np.asarray(inp["g_final"])[None, :], (128, D)))
    s["w_rt"] = f(np.concatenate([np.asarray(inp["w_router_grp"])[0], np.asarray(inp["w_router_exp"])[0]], axis=1))
    brt = np.concatenate([np.asarray(inp["b_router_grp"])[0], np.asarray(inp["b_router_exp"])[0]])
    s["b_rt"] = f(np.broadcast_to(brt[None, :], (128, 72)))
    s["w_gate"] = f(inp["w_gate"][0])
    s["w_up"] = f(inp["w_up"][0])
    s["w_down"] = f(inp["w_down"][0])
    lam_re = np.asarray(inp["ssm_lam_re"])[0]
    lam_im = np.asarray(inp["ssm_lam_im"])[0]
    ls = np.asarray(inp["ssm_log_step"])[0]
    b_re = np.asarray(inp["ssm_b_re"])[0]
    b_im = np.asarray(inp["ssm_b_im"])[0]
    c_re = np.asarray(inp["ssm_c_re"])[0]
    c_im = np.asarray(inp["ssm_c_im"])[0]
    dsk = np.asarray(inp["ssm_d"])[0]

    def oct_rep(a):
        return f(np.repeat(a.reshape(16, 8, 1, 64), 16, axis=2).transpose(1, 2, 0, 3).reshape(128, 16, 64))
    s["s_lr"] = oct_rep(lam_re)
    s["s_li"] = oct_rep(lam_im)
    s["s_ls"] = f(np.repeat(ls.reshape(16, 8, 1), 16, axis=2).transpose(1, 2, 0).reshape(128, 16))
    s["s_br"] = f(b_re.reshape(16, 8, 64, 16).transpose(1, 3, 0, 2).reshape(128, 16, 64))
    s["s_bi"] = f(b_im.reshape(16, 8, 64, 16).transpose(1, 3, 0, 2).reshape(128, 16, 64))
    s["s_d"] = f(dsk.reshape(16, 8, 16).transpose(1, 2, 0).reshape(128, 16))
    s["s_crT"] = f(c_re.transpose(2, 0, 1).reshape(64, G * 16))
    s["s_ciT"] = f(c_im.transpose(2, 0, 1).reshape(64, G * 16))
    s["s_liT"] = f(np.concatenate([lam_im.T, lam_im.T], axis=0))
    s["s_lrT"] = f(np.concatenate([lam_re.T, lam_re.T], axis=0))
    s["s_lsT"] = f(np.broadcast_to(ls[None, :], (128, G)))
    return s


def make_in_maps(inp, cores=range(8)):
    shared = _shared_inputs(inp)
    x = np.asarray(inp["x"], dtype=np.float32)
    cs = [_consts(0), _consts(1)]
    maps = []
    for core in cores:
        b, half = core // 2, core % 2
        m = dict(shared)
        m.update(cs[half])
        if half == 0:
            xin = np.concatenate([np.zeros((TOK, D), np.float32), x[b, 0:TOK]], axis=0)
        else:
            xin = x[b]
        m["xin"] = np.ascontiguousarray(xin)
        maps.append(m)
    return maps


def kernel(**inputs):
    nc = build()
    in_maps = make_in_maps(inputs)
    res = run_bass_kernel_spmd(nc, in_maps, core_ids=list(range(8)))
    outp = np.zeros((4, 2048, D), np.float32)
    for core in range(8):
        b, half = core // 2, core % 2
        outp[b, half * TOK:(half + 1) * TOK, :] = res.results[core]["out"]
    return outp
```
